# Optimizing a Trainium2 kernel written in Bass

```python
import numpy as np
import jax
import jax.numpy as jnp
from jax import lax


D_MODEL = 1024
BATCH = 8
SEQ = 4096
DEPTH = 4

HEAD_DIM = D_MODEL // 16
N_RET = 6
N_FOX = 6
N_NSA = 4
RET_W = N_RET * HEAD_DIM
FOX_W = N_FOX * HEAD_DIM
NSA_W = N_NSA * HEAD_DIM
D_MIX = RET_W + FOX_W + NSA_W
D_FF = 4 * D_MODEL
RMS_EPS = 1e-6
BLOCK_Q = 128
RET_CHUNK = 128
ROPE_BASE = 10000.0
NSA_CMP_LEN = 32
NSA_CMP_STRIDE = 16
NSA_CMP_HIDDEN = 4 * HEAD_DIM
NSA_SEL_LEN = 64
NSA_TOP_N = 16
NSA_WINDOW = 512
NSA_FORCED_SCORE = 1e4
NEG_INF = -1e30
IN_SPLIT_SIZES = (RET_W, RET_W, RET_W, RET_W, FOX_W, FOX_W, FOX_W, N_FOX, NSA_W, HEAD_DIM, HEAD_DIM, HEAD_DIM, HEAD_DIM, HEAD_DIM, HEAD_DIM, 3 * N_NSA)
IN_COLS = 4 * RET_W + 3 * FOX_W + N_FOX + NSA_W + 6 * HEAD_DIM + 3 * N_NSA

kernel_name = 'hybrid_retention_fox_nsa_trunk'


def _rms_norm(x, g):
    xf = x.astype(jnp.float32)
    y = xf * lax.rsqrt(jnp.mean(xf * xf, axis=-1, keepdims=True) + RMS_EPS)
    return (y * g.astype(jnp.float32)).astype(x.dtype)


def _head_rms_norm(y, g, n_heads):
    B, S, W = y.shape
    yh = y.reshape(B, S, n_heads, W // n_heads)
    yh = yh * lax.rsqrt(jnp.mean(yh * yh, axis=-1, keepdims=True) + RMS_EPS)
    return yh.reshape(B, S, W) * g.astype(jnp.float32)


def _head_layer_norm(y, g, n_heads):
    B, S, W = y.shape
    yh = y.reshape(B, S, n_heads, W // n_heads)
    yc = yh - jnp.mean(yh, axis=-1, keepdims=True)
    yh = yc * lax.rsqrt(jnp.mean(yc * yc, axis=-1, keepdims=True) + RMS_EPS)
    return yh.reshape(B, S, W) * g.astype(jnp.float32)


def _to_heads(t, n_heads):
    B, S, _ = t.shape
    return t.astype(jnp.float32).reshape(B, S, n_heads, HEAD_DIM).transpose(0, 2, 1, 3)


def _to_blocks(t, block):
    B, H, S = t.shape[:3]
    t = t.reshape((B, H, S // block, block) + t.shape[3:])
    return jnp.moveaxis(t, 2, 0)


def _from_blocks(o, width):
    NB, B, H, BQ, D = o.shape
    return o.transpose(1, 0, 3, 2, 4).reshape(B, NB * BQ, width)


def _rotary(t):
    S = t.shape[2]
    half = HEAD_DIM // 2
    inv = 1.0 / (ROPE_BASE ** (jnp.arange(half, dtype=jnp.float32) / half))
    ang = jnp.arange(S, dtype=jnp.float32)[:, None] * inv[None, :]
    cos, sin = jnp.cos(ang), jnp.sin(ang)
    t1, t2 = t[..., :half], t[..., half:]
    return jnp.concatenate([t1 * cos - t2 * sin, t1 * sin + t2 * cos], axis=-1)


def _masked_softmax(s, mask):
    p = jax.nn.softmax(jnp.where(mask, s, NEG_INF), axis=-1)
    return p * mask.astype(jnp.float32)


def _retention(q, k, v, g, gain):
    B, S, _ = q.shape
    q = _rotary(_to_heads(q, N_RET))
    k = _rotary(_to_heads(k, N_RET)) * (HEAD_DIM ** -0.5)
    v = _to_heads(v, N_RET)
    log_gamma = jnp.log(1.0 - 2.0 ** (-5.0 - jnp.arange(N_RET, dtype=jnp.float32)))
    C = RET_CHUNK
    idx = jnp.arange(C, dtype=jnp.float32)
    diff = idx[:, None] - idx[None, :]
    decay_in = jnp.where(diff >= 0, jnp.exp(log_gamma[:, None, None] * jnp.maximum(diff, 0.0)), 0.0)
    decay_q = jnp.exp(log_gamma[:, None] * (idx + 1.0))[..., None]
    decay_k = jnp.exp(log_gamma[:, None] * (C - 1.0 - idx))[..., None]
    decay_chunk = jnp.exp(log_gamma * C)[:, None, None]

    def step(state, qkv):
        qc, kc, vc = qkv
        inner = jnp.einsum('bhnd,bhmd->bhnm', qc, kc) * decay_in
        out = jnp.einsum('bhnm,bhme->bhne', inner, vc) + jnp.einsum('bhnd,bhde->bhne', qc, state) * decay_q
        state = state * decay_chunk + jnp.einsum('bhmd,bhme->bhde', kc * decay_k, vc)
        return state, out

    state0 = jnp.zeros((B, N_RET, HEAD_DIM, HEAD_DIM), jnp.float32)
    _, out = lax.scan(step, state0, (_to_blocks(q, C), _to_blocks(k, C), _to_blocks(v, C)))
    out = _head_layer_norm(_from_blocks(out, RET_W), gain, N_RET)
    return out * jax.nn.silu(g.astype(jnp.float32))


def _forgetting_attention(q, k, v, f_logit, f_bias, gain):
    B, S, _ = q.shape
    q = _to_heads(q, N_FOX) * (HEAD_DIM ** -0.5)
    k = _to_heads(k, N_FOX)
    v = _to_heads(v, N_FOX)
    log_f = jax.nn.log_sigmoid(f_logit.astype(jnp.float32) + f_bias.astype(jnp.float32))
    cum = jnp.cumsum(log_f, axis=1).transpose(0, 2, 1)
    NB = S // BLOCK_Q
    key_pos = jnp.arange(S)

    def block(args):
        qb, cb, start = args
        t = start + jnp.arange(BLOCK_Q)
        s = jnp.einsum('bhqd,bhkd->bhqk', qb, k) + (cb[..., None] - cum[:, :, None, :])
        s = jnp.where(key_pos[None, :] <= t[:, None], s, NEG_INF)
        p = jax.nn.softmax(s, axis=-1)
        return jnp.einsum('bhqk,bhkd->bhqd', p, v)

    out = lax.map(block, (_to_blocks(q, BLOCK_Q), _to_blocks(cum, BLOCK_Q), jnp.arange(NB) * BLOCK_Q))
    return _head_rms_norm(_from_blocks(out, FOX_W), gain, N_FOX)


def _nsa_compress(kv, pos_emb, w1, w2):
    B, S, D = kv.shape
    n_cmp = (S - NSA_CMP_LEN) // NSA_CMP_STRIDE + 1
    idx = (jnp.arange(n_cmp) * NSA_CMP_STRIDE)[:, None] + jnp.arange(NSA_CMP_LEN)[None, :]
    blocks = kv[:, idx] + pos_emb.astype(jnp.float32)
    hid = jax.nn.gelu(blocks.reshape(B, n_cmp, NSA_CMP_LEN * D) @ w1)
    return (hid @ w2).astype(jnp.float32)


def _nsa_attention(q, k_cmp, v_cmp, k_sel, v_sel, k_win, v_win, gate_logit, pos_k, pos_v, w1_k, w2_k, w1_v, w2_v, gain):
    B, S, _ = q.shape
    q = _to_heads(q, N_NSA) * (HEAD_DIM ** -0.5)
    f32 = jnp.float32
    kc = _nsa_compress(k_cmp.astype(f32), pos_k, w1_k, w2_k)
    vc = _nsa_compress(v_cmp.astype(f32), pos_v, w1_v, w2_v)
    n_cmp = kc.shape[1]
    cmp_end = jnp.arange(n_cmp) * NSA_CMP_STRIDE + NSA_CMP_LEN - 1
    n_sel = S // NSA_SEL_LEN
    top_n = min(NSA_TOP_N, n_sel)
    cs = np.arange(n_cmp) * NSA_CMP_STRIDE
    ss = np.arange(n_sel) * NSA_SEL_LEN
    overlap = np.clip(np.minimum(cs[:, None] + NSA_CMP_LEN, ss[None, :] + NSA_SEL_LEN) - np.maximum(cs[:, None], ss[None, :]), 0, None)
    cmp_to_sel = jnp.asarray(overlap / NSA_CMP_LEN, dtype=f32)
    ks_blocks = k_sel.astype(f32).reshape(B, n_sel, NSA_SEL_LEN, HEAD_DIM)
    vs_blocks = v_sel.astype(f32).reshape(B, n_sel, NSA_SEL_LEN, HEAD_DIM)
    win_len = NSA_WINDOW + BLOCK_Q
    kw_pad = jnp.pad(k_win.astype(f32), ((0, 0), (NSA_WINDOW, 0), (0, 0)))
    vw_pad = jnp.pad(v_win.astype(f32), ((0, 0), (NSA_WINDOW, 0), (0, 0)))
    gates = jax.nn.sigmoid(gate_logit.astype(f32)).reshape(B, S, N_NSA, 3).transpose(0, 2, 1, 3)
    sel_ids = jnp.arange(n_sel)
    NB = S // BLOCK_Q

    def block(args):
        qb, gb, start = args
        t = start + jnp.arange(BLOCK_Q)
        p_c = _masked_softmax(jnp.einsum('bhqd,bnd->bhqn', qb, kc), cmp_end[None, :] <= t[:, None])
        o_c = jnp.einsum('bhqn,bnd->bhqd', p_c, vc)
        imp = jnp.einsum('bhqn,ns->bqs', p_c, cmp_to_sel)
        cur = t // NSA_SEL_LEN
        visible = sel_ids[None, :] * NSA_SEL_LEN <= t[:, None]
        forced = (sel_ids[None, :] == 0) | (sel_ids[None, :] == cur[:, None]) | (sel_ids[None, :] == cur[:, None] - 1)
        score = jnp.where(forced[None], NSA_FORCED_SCORE, imp)
        score = jnp.where(visible[None], score, -1.0)
        _, sel = lax.top_k(score, top_n)
        gk = jax.vmap(lambda kb, ids: kb[ids])(ks_blocks, sel)
        gv = jax.vmap(lambda vb, ids: vb[ids])(vs_blocks, sel)
        tok = sel[..., None] * NSA_SEL_LEN + jnp.arange(NSA_SEL_LEN)
        mask_s = (tok <= t[None, :, None, None]).reshape(B, 1, BLOCK_Q, top_n * NSA_SEL_LEN)
        s_s = jnp.einsum('bhqd,bqnld->bhqnl', qb, gk).reshape(B, N_NSA, BLOCK_Q, top_n * NSA_SEL_LEN)
        p_s = _masked_softmax(s_s, mask_s)
        o_s = jnp.einsum('bhqm,bqmd->bhqd', p_s, gv.reshape(B, BLOCK_Q, top_n * NSA_SEL_LEN, HEAD_DIM))
        kw = lax.dynamic_slice_in_dim(kw_pad, start, win_len, axis=1)
        vw = lax.dynamic_slice_in_dim(vw_pad, start, win_len, axis=1)
        kpos = start - NSA_WINDOW + jnp.arange(win_len)
        mask_w = (kpos[None, :] >= 0) & (kpos[None, :] <= t[:, None]) & (kpos[None, :] > t[:, None] - NSA_WINDOW)
        p_w = _masked_softmax(jnp.einsum('bhqd,bkd->bhqk', qb, kw), mask_w)
        o_w = jnp.einsum('bhqk,bkd->bhqd', p_w, vw)
        return gb[..., 0:1] * o_c + gb[..., 1:2] * o_s + gb[..., 2:3] * o_w

    out = lax.map(block, (_to_blocks(q, BLOCK_Q), _to_blocks(gates, BLOCK_Q), jnp.arange(NB) * BLOCK_Q))
    return _head_rms_norm(_from_blocks(out, NSA_W), gain, N_NSA)


def setup_inputs(seed: int = 0) -> dict:
    key = jax.random.key(seed)
    ks = jax.random.split(key, 20)
    f32 = jnp.float32

    def nrm(k, shape, scale):
        return jax.random.normal(k, shape, f32) * scale

    def gain(k, shape):
        return 1.0 + 0.1 * jax.random.normal(k, shape, f32)

    return {
        'x': jax.random.normal(ks[0], (BATCH, SEQ, D_MODEL), f32),
        'norm_attn': gain(ks[1], (DEPTH, D_MODEL)),
        'w_in': nrm(ks[2], (DEPTH, D_MODEL, IN_COLS), D_MODEL ** -0.5),
        'fox_forget_bias': 1.0 + 2.0 * jax.random.uniform(ks[3], (DEPTH, N_FOX), f32),
        'ret_norm_gain': gain(ks[4], (DEPTH, RET_W)),
        'fox_norm_gain': gain(ks[5], (DEPTH, FOX_W)),
        'nsa_norm_gain': gain(ks[6], (DEPTH, NSA_W)),
        'nsa_cmp_pos_k': nrm(ks[7], (DEPTH, NSA_CMP_LEN, HEAD_DIM), 0.2),
        'nsa_cmp_pos_v': nrm(ks[8], (DEPTH, NSA_CMP_LEN, HEAD_DIM), 0.2),
        'nsa_cmp_w1_k': nrm(ks[9], (DEPTH, NSA_CMP_LEN * HEAD_DIM, NSA_CMP_HIDDEN), (NSA_CMP_LEN * HEAD_DIM) ** -0.5),
        'nsa_cmp_w2_k': nrm(ks[10], (DEPTH, NSA_CMP_HIDDEN, HEAD_DIM), NSA_CMP_HIDDEN ** -0.5),
        'nsa_cmp_w1_v': nrm(ks[11], (DEPTH, NSA_CMP_LEN * HEAD_DIM, NSA_CMP_HIDDEN), (NSA_CMP_LEN * HEAD_DIM) ** -0.5),
        'nsa_cmp_w2_v': nrm(ks[12], (DEPTH, NSA_CMP_HIDDEN, HEAD_DIM), NSA_CMP_HIDDEN ** -0.5),
        'w_out': nrm(ks[13], (DEPTH, D_MIX, D_MODEL), D_MIX ** -0.5),
        'norm_mlp': gain(ks[14], (DEPTH, D_MODEL)),
        'w_mlp_in': nrm(ks[15], (DEPTH, D_MODEL, D_FF), D_MODEL ** -0.5),
        'w_mlp_out': nrm(ks[16], (DEPTH, D_FF, D_MODEL), D_FF ** -0.5),
        'norm_final': gain(ks[17], (D_MODEL,)),
    }


def reference(x, norm_attn, w_in, fox_forget_bias, ret_norm_gain, fox_norm_gain, nsa_norm_gain, nsa_cmp_pos_k, nsa_cmp_pos_v, nsa_cmp_w1_k, nsa_cmp_w2_k, nsa_cmp_w1_v, nsa_cmp_w2_v, w_out, norm_mlp, w_mlp_in, w_mlp_out, norm_final):
    split_points = np.cumsum(IN_SPLIT_SIZES)[:-1].tolist()
    for l in range(DEPTH):
        h = _rms_norm(x, norm_attn[l])
        parts = jnp.split(h @ w_in[l], split_points, axis=-1)
        (rq, rk, rv, rg, fq, fk, fv, ff, nq, nkc, nvc, nks, nvs, nkw, nvw, ngate) = parts
        y_ret = _retention(rq, rk, rv, rg, ret_norm_gain[l])
        y_fox = _forgetting_attention(fq, fk, fv, ff, fox_forget_bias[l], fox_norm_gain[l])
        y_nsa = _nsa_attention(nq, nkc, nvc, nks, nvs, nkw, nvw, ngate, nsa_cmp_pos_k[l], nsa_cmp_pos_v[l], nsa_cmp_w1_k[l], nsa_cmp_w2_k[l], nsa_cmp_w1_v[l], nsa_cmp_w2_v[l], nsa_norm_gain[l])
        y = jnp.concatenate([y_ret, y_fox, y_nsa], axis=-1).astype(x.dtype)
        x = x + y @ w_out[l]
        h = _rms_norm(x, norm_mlp[l])
        x = x + jnp.square(jax.nn.relu(h @ w_mlp_in[l])) @ w_mlp_out[l]
    return _rms_norm(x, norm_final)
```

```python
import contextlib
import numpy as np
import ml_dtypes
import concourse.bass as bass
import concourse.mybir as mybir
from concourse.bass_utils import run_bass_kernel_spmd

F32 = mybir.dt.float32
BF16 = mybir.dt.bfloat16
AF = mybir.ActivationFunctionType
ALU = mybir.AluOpType
AX = mybir.AxisListType

S = 4096
D = 1024
NL = 4
EPS = 1e-6
NEG = -30000.0
TM_RQ, TM_RK, TM_RV, TM_NVS, TM_NVW, TM_RG, TM_GATE, TM_FV, TM_END = 0, 384, 768, 1152, 1216, 1280, 1664, 1676, 2060
FM0 = TM_END
FM_FQ, FM_FK, FM_NQ, FM_NKC, FM_NVC, FM_NKS, FM_NKW, FM_FF, FM_END = 0, 384, 768, 1024, 1088, 1152, 1216, 1280, 1286
WCOLS = TM_END + FM_END


class Tok:
    def __init__(self, sem, inc):
        self.sem, self.inc, self.count = sem, inc, 0


class Buf:
    def __init__(self, name):
        self.name, self.w, self.r, self.dtok, self.excl = name, {}, {}, None, False


class T:
    def __init__(self, t, name):
        self.t, self.b = t, Buf(name)

    def __getitem__(self, k):
        return self.t[k]


class Eng:
    def __init__(self, name, eng, tok):
        self.name, self.eng, self.tok, self.seen = name, eng, tok, {}


def _b(x):
    return x.b if isinstance(x, T) else x


class KB:
    def __init__(self, nc, es):
        self.nc, self.es = nc, es
        self.free_dtoks = []
        self.all_toks = []
        mk = lambda n: self._newtok(n, 1)
        self.pe = Eng("pe", nc.tensor, mk("s_pe"))
        self.act = Eng("act", nc.scalar, mk("s_act"))
        self.dve = Eng("dve", nc.vector, mk("s_dve"))
        self.pool = Eng("pool", nc.gpsimd, mk("s_pool"))
        self.sp = Eng("sp", nc.sync, None)
        self.engs = [self.pe, self.act, self.dve, self.pool, self.sp]
        self.nsem = 4
        self.nops = 0
        import os
        self.limit = int(os.environ.get("KLIMIT", "1000000000"))

    def _newtok(self, name, inc):
        sem = self.es.enter_context(self.nc.semaphore(name))
        t = Tok(sem, inc)
        self.all_toks.append(t)
        return t

    def _sync(self, E, reads, writes, is_dma=False):
        deps = {}

        def add(tok, c, kind):
            if (not is_dma) and tok is E.tok:
                if kind == "war" or E.name == "pe":
                    return
            if deps.get(tok, 0) < c:
                deps[tok] = c

        for x in reads:
            b = _b(x)
            for tok, c in b.w.items():
                add(tok, c, "raw")
            if b.excl:
                for tok, c in b.r.items():
                    if tok is not E.tok:
                        add(tok, c, "rar")
        for x in writes:
            b = _b(x)
            for tok, c in b.w.items():
                add(tok, c, "waw")
            for tok, c in b.r.items():
                add(tok, c, "war")
        for tok, c in deps.items():
            if E.seen.get(tok, 0) < c:
                E.eng.wait_ge(tok.sem, c * tok.inc)
                E.seen[tok] = c

    def _mark(self, tok, reads, writes):
        for x in reads:
            _b(x).r[tok] = tok.count
        for x in writes:
            _b(x).w[tok] = tok.count

    def op(self, E, reads, writes, fn):
        self.nops += 1
        if self.nops > self.limit:
            return None
        self._sync(E, reads, writes)
        ins = fn(E.eng)
        E.tok.count += 1
        ins.then_inc(E.tok.sem, 1)
        E.seen[E.tok] = max(E.seen.get(E.tok, 0), 0)
        self._mark(E.tok, reads, writes)
        return ins

    def dma(self, out_ap, in_ap, reads, writes, Q=None, **kw):
        self.nops += 1
        if self.nops > self.limit:
            return None
        Q = Q or self.sp
        dst = _b(writes[0])
        if dst.dtok is None:
            if self.free_dtoks:
                dst.dtok = self.free_dtoks.pop()
            else:
                self.nsem += 1
                dst.dtok = self._newtok("s_d%d" % self.nsem, 16)
        self._sync(Q, reads, writes, is_dma=True)
        ins = Q.eng.dma_start(out=out_ap, in_=in_ap, **kw)
        tok = dst.dtok
        tok.count += 1
        ins.then_inc(tok.sem, 16)
        self._mark(tok, reads, writes)

    def barrier(self):
        for E in self.engs:
            for tok in self.all_toks:
                if tok.count > 0 and E.seen.get(tok, 0) < tok.count and not (tok is E.tok and E.name == "pe" and False):
                    E.eng.wait_ge(tok.sem, tok.count * tok.inc)
                    E.seen[tok] = tok.count

    @contextlib.contextmanager
    def phase(self):
        ph = Phase(self)
        with ph.es:
            yield ph
            self.barrier()
        for t in ph.tensors:
            if t.b.dtok is not None:
                self.free_dtoks.append(t.b.dtok)
                t.b.dtok = None


class Phase:
    def __init__(self, kb):
        self.kb, self.es, self.tensors = kb, contextlib.ExitStack(), []

    def sb(self, name, shape, dt):
        self.kb.uid = getattr(self.kb, "uid", 0) + 1
        name = "%s_u%d" % (name, self.kb.uid)
        t = T(self.es.enter_context(self.kb.nc.sbuf_tensor(name, list(shape), dt)), name)
        self.tensors.append(t)
        return t


def make_consts():
    c = {}
    bf = ml_dtypes.bfloat16
    inv = (1.0 / (np.float32(10000.0) ** (np.arange(32, dtype=np.float32) / np.float32(32)))).astype(np.float32)
    ang = (np.arange(S, dtype=np.float32)[:, None] * inv[None, :]).astype(np.float32).astype(np.float64)
    tm = lambda a: np.ascontiguousarray(a.reshape(32, 128, -1).transpose(1, 0, 2))
    c["c_cos"] = tm(np.cos(ang).astype(np.float32))
    c["c_sin"] = tm(np.sin(ang).astype(np.float32))
    lg = np.log(1.0 - 2.0 ** (-5.0 - np.arange(6, dtype=np.float64)))
    idx = np.arange(128, dtype=np.float64)
    dec = np.zeros((128, 6, 128), np.float64)
    for h in range(6):
        diff = idx[None, :] - idx[:, None]
        dec[:, h, :] = np.where(diff >= 0, np.exp(lg[h] * np.maximum(diff, 0)), 0.0) * 0.125
    c["c_dec"] = dec.astype(np.float32)
    dq = np.zeros((64, 6, 128), np.float64)
    for h in range(6):
        dq[:, h, :] = np.exp(lg[h] * (idx + 1.0))[None, :]
    c["c_dq"] = dq.astype(np.float32)
    c["c_dk"] = (np.exp(lg[None, :] * (127.0 - idx[:, None])) * 0.125).astype(np.float32)
    c["c_identb"] = np.eye(128, dtype=np.float32).astype(bf)
    c["c_identf"] = np.eye(128, dtype=np.float32)
    sel = np.zeros((128, 128), np.float32)
    sel[127, :] = 1.0
    c["c_sel127"] = sel
    c["c_onesn"] = np.full((128, 128), 1.0 / 1024.0, np.float32).astype(bf)
    s_ = np.arange(128)[:, None, None]
    i_ = np.arange(4)[None, :, None]
    t_ = np.arange(512)[None, None, :]
    c["c_fmask"] = np.where(128 * i_ + s_ <= t_, 0.0, NEG).astype(np.float32).astype(bf)
    tl = np.arange(128)[None, None, :]
    c["c_tri"] = np.broadcast_to(np.where(s_ <= tl, 0.0, NEG), (128, 4, 128)).astype(np.float32).astype(bf)
    c["c_tri2"] = np.broadcast_to(np.where(s_ > tl, 0.0, NEG), (128, 4, 128)).astype(np.float32).astype(bf)
    tok = np.arange(S)
    c["c_expand"] = (tok[None, :] // 64 == np.arange(64)[:, None]).astype(np.float32).astype(bf)
    n_cmp = 255
    cs = np.arange(n_cmp) * 16
    ss = np.arange(64) * 64
    ov = np.clip(np.minimum(cs[:, None] + 32, ss[None, :] + 64) - np.maximum(cs[:, None], ss[None, :]), 0, None)
    M = np.zeros((256, 64), np.float32)
    M[:255] = ov / 32.0
    c["c_cmp2sel"] = np.ascontiguousarray(M.reshape(2, 128, 64).transpose(1, 0, 2)).astype(bf)
    n_all = np.arange(256)
    cm = ((16 * n_all[:, None] + 31 <= tok[None, :]) & (n_all[:, None] < 255)).astype(np.float32)
    c["c_cmpmask"] = np.ascontiguousarray(cm.reshape(2, 128, S).transpose(1, 0, 2)).astype(bf)
    addc = np.zeros((S, 64), np.float32)
    mult = np.ones((S, 64), np.float32)
    sid = np.arange(64)
    for t in range(S):
        cur = t // 64
        inv_ = sid > cur
        addc[t, inv_] = -1.0 - 0.01 * sid[inv_]
        mult[t, inv_] = 0.0
        for s_f, val in ((cur - 1, 3e4), (cur, 2e4), (0, 1e4)):
            if s_f >= 0:
                addc[t, s_f] = val
                mult[t, s_f] = 0.0
    c["c_addc"] = tm(addc)
    c["c_mult"] = tm(mult)
    return c


CONST_SPECS = None


def build(n_layers=NL, debug=False, final_norm=True):
    global CONST_SPECS
    consts = make_consts()
    CONST_SPECS = consts
    nc = bass.Bass("TRN2", target_bir_lowering=False)
    es = contextlib.ExitStack()
    dbg_kind = "ExternalOutput" if debug else "Internal"

    def din(name, shape, dt=F32):
        return nc.dram_tensor(name, list(shape), dt, kind="ExternalInput").ap()

    def dscr(name, shape, dt, kind=None):
        return T(nc.dram_tensor(name, list(shape), dt, kind=kind or dbg_kind).ap(), name)

    xT_in = T(din("xT", [D, S]), "xT")
    w_in = din("w_in", [NL, D, WCOLS])
    w_out = din("w_out", [NL, D, D])
    w_m1 = din("w_m1", [NL, D, 4 * D])
    w_m2 = din("w_m2", [NL, 4 * D, D])
    w1k = din("w1k", [NL, 2048, 256])
    w1v = din("w1v", [NL, 2048, 256])
    w2k = din("w2k", [NL, 256, 64])
    w2v = din("w2v", [NL, 256, 64])
    g_attn = din("g_attn", [NL, 128, 8])
    g_mlp = din("g_mlp", [NL, 128, 8])
    g_fin = din("g_fin", [128, 8])
    gains = din("gains", [NL, 128, 1024])
    fbias = din("fbias", [NL, 6, 1])
    posk = din("posk", [NL, 64, 32])
    posv = din("posv", [NL, 64, 32])
    cin = {k: din(k, v.shape, BF16 if v.dtype == ml_dtypes.bfloat16 else F32) for k, v in consts.items()}

    out_T = T(nc.dram_tensor("outT", [D, S], F32, kind="ExternalOutput").ap(), "outT")
    xA = dscr("xA", [D, S], F32)
    x1 = dscr("x1", [D, S], F32)
    FMd = dscr("FMd", [1280, S], BF16)
    VTM = dscr("VTM", [S, 520], BF16)
    Yd = dscr("Yd", [S, 1024], BF16)
    DQd = dscr("DQd", [2, 6, S], BF16)

    if debug:
        dbg_lf = dscr("dbg_lf", [6, S], F32)
        dbg_gates = dscr("dbg_gates", [128, 32, 12], F32)

    with es:
        kb = KB(nc, es)
        pe, act, dve, pool = kb.pe, kb.act, kb.dve, kb.pool
        ps = [T(es.enter_context(nc.psum_tensor("ps%d" % i, [128, 512], F32)), "ps%d" % i) for i in range(6)]
        psb = T(es.enter_context(nc.psum_tensor("psb", [128, 1024], BF16)), "psb")
        psb2 = T(es.enter_context(nc.psum_tensor("psb2", [128, 1024], BF16)), "psb2")
        for t_ in ps + [psb, psb2]:
            t_.b.excl = True

        pers = Phase(kb)
        es.enter_context(pers.es)
        identb = pers.sb("identb", [128, 128], BF16)
        identf = pers.sb("identf", [128, 128], F32)
        onesn = pers.sb("onesn", [128, 128], BF16)
        for tname, t in (("c_identb", identb), ("c_identf", identf), ("c_onesn", onesn)):
            kb.dma(t[:], cin[tname][:, :], [], [t])

        def load_cast(ph, dst, dst_ap_fn, src_ap_fn, nparts, ncols, nk, stage, engines):
            for k in range(nk):
                st = stage[k % len(stage)]
                kb.dma(st[0:nparts, 0:ncols], src_ap_fn(k), [], [st])
                E = engines[k % len(engines)]
                if E is act:
                    kb.op(E, [st], [dst], lambda e: e.activation(out=dst_ap_fn(k), in_=st[0:nparts, 0:ncols], func=AF.Copy))
                else:
                    kb.op(E, [st], [dst], lambda e: e.tensor_copy(out=dst_ap_fn(k), in_=st[0:nparts, 0:ncols]))

        epsc = pers.sb("epsc", [128, 1], F32)
        kb.op(dve, [], [epsc], lambda e: e.memset(epsc[:], EPS))

        def rsqrt(oT, o_ap, iT, i_ap, scale):
            npart = o_ap.shape[0]
            kb.op(act, [iT, epsc], [oT], lambda e: e.activation(out=o_ap, in_=i_ap, func=AF.Ln, bias=epsc[0:npart, :], scale=scale))
            kb.op(act, [oT], [oT], lambda e: e.activation(out=o_ap, in_=o_ap, func=AF.Exp, scale=-0.5))

        def head_finish(o3, nb, on, osq, rinv, ss, gain_ap, yT, y_ap, srcT, gT):
            kb.op(dve, [srcT], [rinv], lambda e: e.reciprocal(out=rinv[:, 0:nb], in_=o3[:, :, 64]))
            kb.op(dve, [srcT, rinv], [on], lambda e: e.tensor_tensor(out=on[:, 0:nb, :], in0=o3[:, :, 0:64], in1=rinv[:, 0:nb].unsqueeze(2).to_broadcast([128, nb, 64]), op=ALU.mult))
            norm_gain(nb, on, osq, ss, gain_ap, yT, y_ap, gT)

        def norm_gain(nb, on, osq, ss, gain_ap, yT, y_ap, gT):
            kb.op(pool, [on], [osq], lambda e: e.tensor_tensor(out=osq[:, 0:nb, :], in0=on[:, 0:nb, :], in1=on[:, 0:nb, :], op=ALU.mult))
            kb.op(dve, [osq], [ss], lambda e: e.tensor_reduce(out=ss[:, 0:nb], in_=osq[:, 0:nb, :], axis=AX.X, op=ALU.add))
            rsqrt(ss, ss[:, 0:nb], ss, ss[:, 0:nb], 1.0 / 64.0)
            kb.op(dve, [on, ss], [on], lambda e: e.tensor_tensor(out=on[:, 0:nb, :], in0=on[:, 0:nb, :], in1=ss[:, 0:nb].unsqueeze(2).to_broadcast([128, nb, 64]), op=ALU.mult))
            kb.op(pool, [on, gT], [yT], lambda e: e.tensor_tensor(out=y_ap, in0=on[:, 0:nb, :], in1=gain_ap, op=ALU.mult))

        def rmsnorm_tile(ph, xt, sq, hT, g_sb, rstd, ntok, psbank):
            for c in range(8):
                E = act if c % 2 == 0 else pool
                if E is act:
                    kb.op(act, [xt], [sq], lambda e: e.activation(out=sq[:, c, 0:ntok], in_=xt[:, c, 0:ntok], func=AF.Square))
                else:
                    kb.op(pool, [xt], [sq], lambda e: e.tensor_tensor(out=sq[:, c, 0:ntok], in0=xt[:, c, 0:ntok], in1=xt[:, c, 0:ntok], op=ALU.mult))
            for c in range(8):
                kb.op(pe, [sq, onesn], [psbank], lambda e: e.matmul(psbank[:, 0:ntok], lhsT=onesn[:, :], rhs=sq[:, c, 0:ntok], start=(c == 0), stop=(c == 7)))
            rsqrt(rstd, rstd[:, 0:ntok], psbank, psbank[:, 0:ntok], 1.0)
            for c in range(8):
                E = dve
                kb.op(E, [xt, rstd, g_sb], [hT], lambda e: e.scalar_tensor_tensor(out=hT[:, c, 0:ntok], in0=xt[:, c, 0:ntok], scalar=g_sb[:, c:c + 1], in1=rstd[:, 0:ntok], op0=ALU.mult, op1=ALU.mult))

        x_cur = xT_in
        for L in range(n_layers):
            with kb.phase() as lay:
                cumTM = lay.sb("cumTM", [128, 32, 6], F32)
                Rend = lay.sb("Rend", [128, 32, 6], F32)
                kcT = lay.sb("kcT", [64, 256], BF16)
                VCM = lay.sb("VCM", [128, 2, 129], BF16)
                gates_sb = lay.sb("gates_sb", [128, 32, 12], F32)
                lfT = lay.sb("lfT", [6, S], F32)
                with kb.phase() as ph:
                    winT = ph.sb("winT", [128, 8, WCOLS], BF16)
                    stage = [ph.sb("wst%d" % i, [128, WCOLS], F32) for i in range(2)]
                    load_cast(ph, winT, lambda k: winT[:, k, :], lambda k: w_in[L, k * 128:(k + 1) * 128, :], 128, WCOLS, 8, stage, [act, dve, pool, act, dve, act, dve, pool])
                    g_sb = ph.sb("g_sb", [128, 8], F32)
                    kb.dma(g_sb[:], g_attn[L], [], [g_sb])
                    gain_r = ph.sb("gain_r", [128, 384], F32)
                    kb.dma(gain_r[:], gains[L, :, 0:384], [], [gain_r])
                    nfb = ph.sb("nfb", [6, 1], F32)
                    kb.dma(nfb[:], fbias[L], [], [nfb])
                    kb.op(dve, [nfb], [nfb], lambda e: e.tensor_scalar(out=nfb[:], in0=nfb[:], scalar1=-1.0, scalar2=None, op0=ALU.mult))
                    cos = ph.sb("cos", [128, 32, 32], F32)
                    sin = ph.sb("sin", [128, 32, 32], F32)
                    dec = ph.sb("dec", [128, 6, 128], F32)
                    dqt = ph.sb("dqt", [64, 6, 128], F32)
                    dkt = ph.sb("dkt", [128, 6], F32)
                    for t, n in ((cos, "c_cos"), (sin, "c_sin"), (dec, "c_dec"), (dqt, "c_dq"), (dkt, "c_dk")):
                        kb.dma(t[:], cin[n], [], [t])
                    xts = [ph.sb("xt%d" % i, [128, 8, 512], F32) for i in range(2)]
                    sq = ph.sb("sq", [128, 8, 512], BF16)
                    hT = ph.sb("hT", [128, 8, 512], BF16)
                    rstd = ph.sb("rstd", [128, 512], F32)
                    fme = [ph.sb("fme%d" % i, [128, 512], BF16) for i in range(3)]
                    lft = ph.sb("lft", [6, 512], F32)
                    vt = [ph.sb("vt%d" % i, [128, 8, 65], BF16) for i in range(2)]
                    for v in vt:
                        kb.op(pool, [], [v], lambda e: e.memset(v[:], 1.0))
                    ra, rb, rc, rd = [ph.sb("r%s" % n, [128, 12, 32], F32) for n in "abcd"]
                    qkr = ph.sb("qkr", [128, 12, 2, 32], BF16)
                    vbf = ph.sb("vbf", [128, 384], BF16)
                    qTp = ph.sb("qTp", [64, 6, 128], BF16)
                    qTd = ph.sb("qTd", [64, 6, 128], BF16)
                    kTp = ph.sb("kTp", [64, 6, 128], BF16)
                    inn = ph.sb("inn", [128, 6, 128], BF16)
                    kd = ph.sb("kd", [128, 6, 64], BF16)
                    Sf = ph.sb("Sf", [64, 6, 64], F32)
                    Sb = ph.sb("Sb", [64, 6, 64], BF16)
                    kb.op(dve, [], [Sf], lambda e: e.memset(Sf[:], 0.0))
                    kb.op(dve, [], [Sb], lambda e: e.memset(Sb[:], 0.0))
                    of = ph.sb("of", [128, 6, 64], F32)
                    osq = ph.sb("osq", [128, 6, 64], F32)
                    sg = ph.sb("sg", [128, 384], F32)
                    st1 = ph.sb("st1", [128, 6], F32)
                    st2 = ph.sb("st2", [128, 6], F32)
                    st3 = ph.sb("st3", [128, 6], F32)
                    yr = [ph.sb("yr%d" % i, [128, 384], BF16) for i in range(2)]

                    def load_x(Tt):
                        xt = xts[Tt % 2]
                        kb.dma(xt[:], x_cur[:, :].rearrange("(c p) t -> p c t", p=128)[:, :, Tt * 512:(Tt + 1) * 512], [x_cur], [xt])

                    load_x(0)
                    for Tt in range(8):
                        xt = xts[Tt % 2]
                        if Tt + 1 < 8:
                            load_x(Tt + 1)
                        rmsnorm_tile(ph, xt, sq, hT, g_sb, rstd, 512, ps[0])
                        def fm_chunk(j, bank):
                            m = 128 if j < 10 else 6
                            for c in range(8):
                                kb.op(pe, [winT, hT], [bank], lambda e: e.matmul(bank[0:m, :], lhsT=winT[:, c, FM0 + j * 128:FM0 + j * 128 + m], rhs=hT[:, c, :], start=(c == 0), stop=(c == 7)))
                            if j < 10:
                                fe = fme[j % 3]
                                scale = 0.125 if (j < 3 or j in (6, 7)) else 1.0
                                kb.op(act, [bank], [fe], lambda e: e.activation(out=fe[:], in_=bank[:, :], func=AF.Copy, scale=scale))
                                kb.dma(FMd[j * 128:(j + 1) * 128, Tt * 512:(Tt + 1) * 512], fe[:], [fe], [FMd])
                            else:
                                kb.op(act, [bank, nfb], [lft], lambda e: e.activation(out=lft[:], in_=bank[0:6, :], func=AF.Exp, bias=nfb[:], scale=-1.0))
                                kb.op(act, [lft], [lfT], lambda e: e.activation(out=lfT[:, Tt * 512:(Tt + 1) * 512], in_=lft[:], func=AF.Ln, bias=1.0, scale=1.0))
                        fm_chunk(0, ps[1])
                        fm_chunk(1, ps[2])
                        for sub in range(4):
                            cc = Tt * 4 + sub
                            tk = slice(sub * 128, (sub + 1) * 128)
                            groups = [(ps[3], TM_RQ, 384), (ps[4], TM_RK, 384), (ps[5], TM_RV, 512), (ps[0], TM_RG, 396), (ps[1 + (sub % 2)], TM_FV, 384)]
                            for bank, c0, ncol in groups:
                                for c in range(8):
                                    kb.op(pe, [winT, hT], [bank], lambda e: e.matmul(bank[:, 0:ncol], lhsT=hT[:, c, tk], rhs=winT[:, c, c0:c0 + ncol], start=(c == 0), stop=(c == 7)))
                            for j_ in {0: (2, 3, 4), 1: (5, 6), 2: (7, 8), 3: (9, 10)}[sub]:
                                fm_chunk(j_, ps[1 + ((sub + 1) % 2)])
                            pq, pk, pv, pg, pf = ps[3], ps[4], ps[5], ps[0], ps[1 + (sub % 2)]
                            v_t = vt[cc % 2]
                            kb.op(act, [pf], [v_t], lambda e: e.activation(out=v_t[:, 0:6, 0:64], in_=pf[:, 0:384].rearrange("p (h d) -> p h d", d=64), func=AF.Copy))
                            kb.op(act, [pv], [v_t], lambda e: e.activation(out=v_t[:, 6:8, 0:64], in_=pv[:, 384:512].rearrange("p (h d) -> p h d", d=64), func=AF.Copy))
                            kb.dma(VTM[cc * 128:(cc + 1) * 128, :], v_t[:].rearrange("p h d -> p (h d)"), [v_t], [VTM])
                            kb.op(act, [pv], [vbf], lambda e: e.activation(out=vbf[:], in_=pv[:, 0:384], func=AF.Copy))
                            kb.op(act, [pg], [gates_sb], lambda e: e.activation(out=gates_sb[:, cc, :], in_=pg[:, 384:396], func=AF.Sigmoid))
                            kb.op(act, [pg], [sg], lambda e: e.activation(out=sg[:], in_=pg[:, 0:384], func=AF.Silu))
                            cosb = cos[:, cc:cc + 1, :].to_broadcast([128, 6, 32])
                            sinb = sin[:, cc:cc + 1, :].to_broadcast([128, 6, 32])
                            for qi, pb in enumerate((pq, pk)):
                                v4 = pb[:, 0:384].rearrange("p (h two f) -> p h two f", two=2, f=32)
                                hs = slice(qi * 6, qi * 6 + 6)
                                kb.op(dve, [pb, cos], [ra], lambda e: e.tensor_tensor(out=ra[:, hs, :], in0=v4[:, :, 0, :], in1=cosb, op=ALU.mult))
                                kb.op(dve, [pb, sin], [rb], lambda e: e.tensor_tensor(out=rb[:, hs, :], in0=v4[:, :, 1, :], in1=sinb, op=ALU.mult))
                                kb.op(dve, [pb, sin], [rc], lambda e: e.tensor_tensor(out=rc[:, hs, :], in0=v4[:, :, 0, :], in1=sinb, op=ALU.mult))
                                kb.op(dve, [pb, cos], [rd], lambda e: e.tensor_tensor(out=rd[:, hs, :], in0=v4[:, :, 1, :], in1=cosb, op=ALU.mult))
                            kb.op(pool, [ra, rb], [qkr], lambda e: e.tensor_tensor(out=qkr[:, :, 0, :], in0=ra[:], in1=rb[:], op=ALU.subtract))
                            kb.op(pool, [rc, rd], [qkr], lambda e: e.tensor_tensor(out=qkr[:, :, 1, :], in0=rc[:], in1=rd[:], op=ALU.add))
                            qk2 = qkr[:].rearrange("p h two f -> p (h two f)")
                            for i in range(12):
                                dstb = psb if i < 6 else psb2
                                kb.op(pe, [qkr, identb], [dstb], lambda e: e.transpose(dstb[0:64, (i % 6) * 128:(i % 6 + 1) * 128], qk2[:, i * 64:(i + 1) * 64], identb[:, :]))
                            pTq = psb[0:64, 0:768].rearrange("p (i n) -> p i n", n=128)
                            pTk = psb2[0:64, 0:768].rearrange("p (i n) -> p i n", n=128)
                            kb.op(act, [psb], [qTp], lambda e: e.activation(out=qTp[:], in_=pTq, func=AF.Copy))
                            kb.op(dve, [psb, dqt], [qTd], lambda e: e.tensor_tensor(out=qTd[:], in0=pTq, in1=dqt[:], op=ALU.mult))
                            kb.op(act, [psb2], [kTp], lambda e: e.activation(out=kTp[:], in_=pTk, func=AF.Copy))
                            for h in range(6):
                                bank = ps[3] if h < 3 else ps[4]
                                kb.op(pe, [kTp, qTp], [bank], lambda e: e.matmul(bank[:, (h % 3) * 128:(h % 3 + 1) * 128], lhsT=kTp[:, h, :], rhs=qTp[:, h, :], start=True, stop=True))
                            for half in range(2):
                                bank = ps[3 + half]
                                kb.op(dve, [bank, dec], [inn], lambda e: e.tensor_tensor(out=inn[:, half * 3:half * 3 + 3, :], in0=bank[:, 0:384].rearrange("p (h n) -> p h n", n=128), in1=dec[:, half * 3:half * 3 + 3, :], op=ALU.mult))
                            po = ps[0]
                            for h in range(6):
                                if cc > 0:
                                    kb.op(pe, [qTd, Sb], [po], lambda e: e.matmul(po[:, h * 64:(h + 1) * 64], lhsT=qTd[:, h, :], rhs=Sb[:, h, :], start=True, stop=False))
                                kb.op(pe, [inn, vbf], [po], lambda e: e.matmul(po[:, h * 64:(h + 1) * 64], lhsT=inn[:, h, :], rhs=vbf[:, h * 64:(h + 1) * 64], start=(cc == 0), stop=True))
                            if cc < 31:
                                kb.op(pool, [qkr, dkt], [kd], lambda e: e.tensor_tensor(out=kd[:], in0=qkr[:, 6:12, :, :].rearrange("p h two f -> p h (two f)"), in1=dkt[:, :].unsqueeze(2).to_broadcast([128, 6, 64]), op=ALU.mult))
                                pst = ps[5]
                                for h in range(6):
                                    kb.op(pe, [kd, vbf], [pst], lambda e: e.matmul(pst[0:64, h * 64:(h + 1) * 64], lhsT=kd[:, h, :], rhs=vbf[:, h * 64:(h + 1) * 64], start=True, stop=True))
                                for h in range(6):
                                    gC = float(np.exp(np.log(1.0 - 2.0 ** (-5.0 - h)) * 128.0))
                                    kb.op(dve, [pst, Sf], [Sf], lambda e: e.scalar_tensor_tensor(out=Sf[:, h, :], in0=Sf[:, h, :], scalar=gC, in1=pst[0:64, h * 64:(h + 1) * 64], op0=ALU.mult, op1=ALU.add))
                                kb.op(pool, [Sf], [Sb], lambda e: e.tensor_copy(out=Sb[:], in_=Sf[:]))
                            kb.op(act, [po], [of], lambda e: e.activation(out=of[:], in_=po[:, 0:384].rearrange("p (h d) -> p h d", d=64), func=AF.Copy))
                            kb.op(dve, [of], [st1], lambda e: e.tensor_reduce(out=st1[:], in_=of[:], axis=AX.X, op=ALU.add))
                            kb.op(pool, [of], [osq], lambda e: e.tensor_tensor(out=osq[:], in0=of[:], in1=of[:], op=ALU.mult))
                            kb.op(dve, [osq], [st2], lambda e: e.tensor_reduce(out=st2[:], in_=osq[:], axis=AX.X, op=ALU.add))
                            kb.op(dve, [st1], [st1], lambda e: e.tensor_scalar(out=st1[:], in0=st1[:], scalar1=1.0 / 64.0, scalar2=None, op0=ALU.mult))
                            kb.op(dve, [st1], [st3], lambda e: e.tensor_tensor(out=st3[:], in0=st1[:], in1=st1[:], op=ALU.mult))
                            kb.op(dve, [st2, st3], [st2], lambda e: e.scalar_tensor_tensor(out=st2[:], in0=st2[:], scalar=1.0 / 64.0, in1=st3[:], op0=ALU.mult, op1=ALU.subtract))
                            rsqrt(st2, st2[:], st2, st2[:], 1.0)
                            kb.op(dve, [of, st1], [of], lambda e: e.tensor_tensor(out=of[:], in0=of[:], in1=st1[:, :].unsqueeze(2).to_broadcast([128, 6, 64]), op=ALU.subtract))
                            kb.op(dve, [of, st2], [of], lambda e: e.tensor_tensor(out=of[:], in0=of[:], in1=st2[:, :].unsqueeze(2).to_broadcast([128, 6, 64]), op=ALU.mult))
                            of2 = of[:].rearrange("p h d -> p (h d)")
                            kb.op(pool, [of, gain_r], [of], lambda e: e.tensor_tensor(out=of2, in0=of2, in1=gain_r[:], op=ALU.mult))
                            y_r = yr[cc % 2]
                            kb.op(pool, [of, sg], [y_r], lambda e: e.tensor_tensor(out=y_r[:], in0=of2, in1=sg[:], op=ALU.mult))
                            kb.dma(Yd[cc * 128:(cc + 1) * 128, 0:384], y_r[:], [y_r], [Yd])
                    if debug:
                        kb.dma(dbg_lf[:, :], lfT[:], [lfT], [dbg_lf])
                        kb.dma(dbg_gates[:, :, :], gates_sb[:], [gates_sb], [dbg_gates])

                if debug == "P":
                    break

                with kb.phase() as ph:
                    cumL = ph.sb("cumL", [6, S], F32)
                    z6 = ph.sb("z6", [6, 512], F32)
                    kb.op(dve, [], [z6], lambda e: e.memset(z6[:], 0.0))
                    for j in range(8):
                        sl = slice(j * 512, (j + 1) * 512)
                        init = 0.0 if j == 0 else cumL[:, j * 512 - 1:j * 512]
                        kb.op(dve, [lfT, z6, cumL], [cumL], lambda e: e.tensor_tensor_scan(out=cumL[:, sl], data0=lfT[:, sl], data1=z6[:], initial=init, op0=ALU.add, op1=ALU.add))
                    dqf = ph.sb("dqf", [6, S], F32)
                    dqh = ph.sb("dqh", [6, S], BF16)
                    dql = ph.sb("dql", [6, S], BF16)
                    for j in range(8):
                        sl = slice(j * 512, (j + 1) * 512)
                        if j == 0:
                            kb.op(dve, [cumL], [dqf], lambda e: e.tensor_scalar(out=dqf[:, sl], in0=cumL[:, sl], scalar1=-1.0, scalar2=None, op0=ALU.mult))
                        else:
                            kb.op(dve, [cumL], [dqf], lambda e: e.tensor_scalar(out=dqf[:, sl], in0=cumL[:, sl], scalar1=-1.0, scalar2=cumL[:, j * 512 - 1:j * 512], op0=ALU.mult, op1=ALU.add))
                    kb.op(dve, [dqf], [dqh], lambda e: e.tensor_copy(out=dqh[:], in_=dqf[:]))
                    kb.op(dve, [dqf, dqh], [dql], lambda e: e.tensor_tensor(out=dql[:], in0=dqf[:], in1=dqh[:], op=ALU.subtract))
                    kb.dma(DQd[0], dqh[:], [dqh], [DQd])
                    kb.dma(DQd[1], dql[:], [dql], [DQd])
                    pc, pr_ = ps[0], ps[1]
                    for c in range(32):
                        kb.op(pe, [cumL, identf], [pc], lambda e: e.transpose(pc[:, c * 6:(c + 1) * 6], cumL[:, c * 128:(c + 1) * 128], identf[0:6, 0:6]))
                        kb.op(pe, [cumL, identf], [pr_], lambda e: e.transpose(pr_[:, c * 6:(c + 1) * 6], cumL[:, c * 128 + 127:c * 128 + 128].to_broadcast([6, 128]), identf[0:6, 0:6]))
                    kb.op(act, [pc], [cumTM], lambda e: e.activation(out=cumTM[:].rearrange("p c h -> p (c h)"), in_=pc[:, 0:192], func=AF.Copy))
                    kb.op(act, [pr_], [Rend], lambda e: e.activation(out=Rend[:].rearrange("p c h -> p (c h)"), in_=pr_[:, 0:192], func=AF.Copy))

                with kb.phase() as ph:
                    kT = [ph.sb("fkT%d" % h, [66, S], BF16) for h in range(6)]
                    for h in range(6):
                        kb.dma(kT[h][0:64, :], FMd[FM_FK + h * 64:FM_FK + (h + 1) * 64, :], [FMd], [kT[h]])
                        kb.op(pool, [], [kT[h]], lambda e: e.memset(kT[h][64:66, :], 1.0))
                    vF = ph.sb("vF", [128, 32, 390], BF16)
                    kb.dma(vF[:], VTM[:, 0:390].rearrange("(c p) f -> p c f", p=128), [VTM], [vF])
                    gain_f = ph.sb("gain_f", [128, 384], F32)
                    kb.dma(gain_f[:], gains[L, :, 384:768], [], [gain_f])
                    fmask = ph.sb("fmask", [128, 4, 512], BF16)
                    kb.dma(fmask[:], cin["c_fmask"], [], [fmask])
                    qts = [ph.sb("fq%d" % i, [66, 6, 512], BF16) for i in range(2)]
                    pts = [ph.sb("fpt%d" % i, [128, 512], BF16) for i in range(3)]
                    ots = [ph.sb("fot%d" % i, [65, 512], F32) for i in range(2)]
                    yts = [ph.sb("fy%d" % i, [128, 4, 384], BF16) for i in range(2)]
                    on = ph.sb("fon", [128, 4, 64], F32)
                    osq = ph.sb("fosq", [128, 4, 64], F32)
                    rinv = ph.sb("frinv", [128, 4], F32)
                    ss = ph.sb("fss", [128, 4], F32)

                    def load_q(j):
                        qt = qts[j % 2]
                        kb.dma(qt[0:64, :, :], FMd[0:384, j * 512:(j + 1) * 512].rearrange("(h d) t -> d h t", d=64), [FMd], [qt])
                        kb.dma(qt[64:66, :, :], DQd[:, :, j * 512:(j + 1) * 512], [DQd], [qt])

                    biases = [ph.sb("fbj%d" % j, [128, 32, 6], F32) for j in range(8)]
                    for j in range(8):
                        nch = 4 * j + 4
                        bj = biases[j]
                        if j == 0:
                            kb.op(pool, [cumTM], [bj], lambda e: e.tensor_copy(out=bj[:, 0:nch, :], in_=cumTM[:, 0:nch, :]))
                        else:
                            kb.op(dve, [cumTM, Rend], [bj], lambda e: e.tensor_tensor(out=bj[:, 0:nch, :], in0=cumTM[:, 0:nch, :], in1=Rend[:, 4 * j - 1:4 * j, :].to_broadcast([128, nch, 6]), op=ALU.subtract))
                    load_q(0)
                    items = [(j, h, c) for j in range(8) for h in range(6) for c in range(4 * j + 4)]
                    n_it = len(items)
                    deferred = []

                    def emit_qk(k):
                        j, h, c = items[k]
                        if h == 0 and c == 0 and j + 1 < 8:
                            load_q(j + 1)
                        qt = qts[j % 2]
                        i = c - 4 * j
                        c0 = 128 * i if i > 0 else 0
                        diag = i >= 0
                        sb_ = ps[1 + (k % 3)]
                        kb.op(pe, [kT[h], qt], [sb_], lambda e: e.matmul(sb_[:, c0:512], lhsT=kT[h][0:66, c * 128:(c + 1) * 128], rhs=qt[0:66, h, c0:512], start=True, stop=not diag))
                        if diag:
                            kb.op(pe, [identb, fmask], [sb_], lambda e: e.matmul(sb_[:, c0:512], lhsT=identb[:, :], rhs=fmask[:, i, c0:512], start=False, stop=True))

                    def finish_b(j, h):
                        ot = ots[h % 2]
                        yt = yts[j % 2]
                        ptr = ps[0]
                        for bq in range(4):
                            kb.op(pe, [ot, identf], [ptr], lambda e: e.transpose(ptr[:, bq * 65:(bq + 1) * 65], ot[0:65, bq * 128:(bq + 1) * 128], identf[0:65, 0:65]))
                        o3 = ptr[:, 0:260].rearrange("p (b f) -> p b f", f=65)
                        head_finish(o3, 4, on, osq, rinv, ss, gain_f[:, h * 64:(h + 1) * 64].unsqueeze(1).to_broadcast([128, 4, 64]), yt, yt[:, :, h * 64:(h + 1) * 64], ptr, gain_f)
                        if h == 5:
                            kb.dma(Yd[j * 512:(j + 1) * 512, 384:768].rearrange("(b p) f -> p b f", p=128), yt[:], [yt], [Yd])

                    def emit_exp_pv(k):
                        j, h, c = items[k]
                        nch = 4 * j + 4
                        i = c - 4 * j
                        c0 = 128 * i if i > 0 else 0
                        sb_ = ps[1 + (k % 3)]
                        pt = pts[k % 3]
                        po = ps[4 + ((j * 6 + h) % 2)]
                        bj = biases[j]
                        kb.op(act, [sb_, bj], [pt], lambda e: e.activation(out=pt[:, c0:512], in_=sb_[:, c0:512], func=AF.Exp, bias=bj[:, c, h:h + 1], scale=1.0))
                        kb.op(pe, [vF, pt], [po], lambda e: e.matmul(po[0:65, c0:512], lhsT=vF[:, c, h * 65:(h + 1) * 65], rhs=pt[:, c0:512], start=(c == 0), stop=(c == nch - 1)))
                        if c == nch - 1:
                            ot = ots[h % 2]
                            kb.op(dve, [po], [ot], lambda e: e.tensor_copy(out=ot[:], in_=po[0:65, :]))
                            deferred.append((k + 2, j, h))

                    emit_qk(0)
                    if n_it > 1:
                        emit_qk(1)
                    for k in range(n_it):
                        if k + 2 < n_it:
                            emit_qk(k + 2)
                        emit_exp_pv(k)
                        while deferred and deferred[0][0] <= k:
                            _, dj, dh = deferred.pop(0)
                            finish_b(dj, dh)
                    while deferred:
                        _, dj, dh = deferred.pop(0)
                        finish_b(dj, dh)
                if debug == "F":
                    break

                with kb.phase() as ph:
                    kb.op(pool, [], [VCM], lambda e: e.memset(VCM[:], 0.0))
                    kb.op(pool, [], [kcT], lambda e: e.memset(kcT[:], 0.0))
                    m_st = ph.sb("m_st", [128, 2, 64], BF16)
                    kb.dma(m_st[:], cin["c_cmp2sel"], [], [m_st])
                    kb.op(pool, [m_st], [VCM], lambda e: e.tensor_copy(out=VCM[:, :, 64:128], in_=m_st[:]))
                    kb.op(pool, [], [VCM], lambda e: e.memset(VCM[:, :, 128:129], 1.0))
                    hid = {}
                    w2s = {}
                    for nm, w1d, w2d, posd, row0 in (("k", w1k, w2k, posk, FM_NKC), ("v", w1v, w2v, posv, FM_NVC)):
                        w1st = ph.sb("w1st" + nm, [64, 32, 256], F32)
                        w1b = ph.sb("w1b" + nm, [64, 32, 256], BF16)
                        kb.dma(w1st[:], w1d[L].rearrange("(l d) j -> d l j", d=64), [], [w1st])
                        kb.op(pool if nm == "k" else dve, [w1st], [w1b], lambda e: e.tensor_copy(out=w1b[:], in_=w1st[:]))
                        w2st = ph.sb("w2st" + nm, [128, 2, 64], F32)
                        w2b = ph.sb("w2b" + nm, [128, 2, 64], BF16)
                        kb.dma(w2st[:], w2d[L].rearrange("(c p) e -> p c e", p=128), [], [w2st])
                        kb.op(dve, [w2st], [w2b], lambda e: e.tensor_copy(out=w2b[:], in_=w2st[:]))
                        w2s[nm] = w2b
                        pos = ph.sb("pos" + nm, [64, 32], F32)
                        kb.dma(pos[:], posd[L], [], [pos])
                        kvT = ph.sb("kvT" + nm, [64, S], BF16)
                        kb.dma(kvT[:], FMd[row0:row0 + 64, :], [FMd], [kvT])
                        blk = ph.sb("blk" + nm, [64, 32, 255], BF16)
                        v3 = kvT[:, :].rearrange("d (i r) -> d i r", r=16)
                        kb.op(dve, [kvT, pos], [blk], lambda e: e.tensor_tensor(out=blk[:, 0:16, :], in0=v3[:, 0:255, :].rearrange("d i r -> d r i"), in1=pos[:, 0:16].unsqueeze(2).to_broadcast([64, 16, 255]), op=ALU.add))
                        kb.op(pool, [kvT, pos], [blk], lambda e: e.tensor_tensor(out=blk[:, 16:32, :], in0=v3[:, 1:256, :].rearrange("d i r -> d r i"), in1=pos[:, 16:32].unsqueeze(2).to_broadcast([64, 16, 255]), op=ALU.add))
                        hb = ph.sb("hid" + nm, [128, 2, 256], BF16)
                        kb.op(pool, [], [hb], lambda e: e.memset(hb[:], 0.0))
                        xs = ph.sb("gx" + nm, [128, 255], F32)
                        x2 = ph.sb("gx2" + nm, [128, 255], F32)
                        for jc in range(2):
                            bank = ps[1 + jc]
                            for l in range(32):
                                kb.op(pe, [w1b, blk], [bank], lambda e: e.matmul(bank[:, 0:255], lhsT=w1b[:, l, jc * 128:(jc + 1) * 128], rhs=blk[:, l, :], start=(l == 0), stop=(l == 31)))
                            kb.op(act, [bank], [xs], lambda e: e.activation(out=xs[:], in_=bank[:, 0:255], func=AF.Copy))
                            kb.op(pool, [xs], [x2], lambda e: e.tensor_tensor(out=x2[:], in0=xs[:], in1=xs[:], op=ALU.mult))
                            kb.op(dve, [x2], [x2], lambda e: e.tensor_scalar(out=x2[:], in0=x2[:], scalar1=0.044715, scalar2=1.0, op0=ALU.mult, op1=ALU.add))
                            kb.op(pool, [x2, xs], [x2], lambda e: e.tensor_tensor(out=x2[:], in0=x2[:], in1=xs[:], op=ALU.mult))
                            kb.op(act, [x2], [x2], lambda e: e.activation(out=x2[:], in_=x2[:], func=AF.Sigmoid, scale=1.5957691216057308))
                            kb.op(pool, [x2, xs], [hb], lambda e: e.tensor_tensor(out=hb[:, jc, 0:255], in0=x2[:], in1=xs[:], op=ALU.mult))
                        hid[nm] = hb
                    pk_ = ps[3]
                    for jc in range(2):
                        kb.op(pe, [w2s["k"], hid["k"]], [pk_], lambda e: e.matmul(pk_[0:64, 0:255], lhsT=w2s["k"][:, jc, :], rhs=hid["k"][:, jc, 0:255], start=(jc == 0), stop=(jc == 1)))
                    kb.op(act, [pk_], [kcT], lambda e: e.activation(out=kcT[:, 0:255], in_=pk_[0:64, 0:255], func=AF.Copy))
                    pv_ = ps[4]
                    for n_ in range(2):
                        nn = 128 if n_ == 0 else 127
                        for jc in range(2):
                            kb.op(pe, [w2s["v"], hid["v"]], [pv_], lambda e: e.matmul(pv_[0:nn, n_ * 64:(n_ + 1) * 64], lhsT=hid["v"][:, jc, n_ * 128:n_ * 128 + nn], rhs=w2s["v"][:, jc, :], start=(jc == 0), stop=(jc == 1)))
                        kb.op(act, [pv_], [VCM], lambda e: e.activation(out=VCM[0:nn, n_, 0:64], in_=pv_[0:nn, n_ * 64:(n_ + 1) * 64], func=AF.Copy))

                with kb.phase() as ph:
                    ksA = ph.sb("ksA", [128, S], BF16)
                    kb.dma(ksA[0:64, :], FMd[FM_NKS:FM_NKS + 64, :], [FMd], [ksA])
                    kb.dma(ksA[64:128, :], cin["c_expand"], [], [ksA])
                    kwT = ph.sb("kwT", [64, S], BF16)
                    kb.dma(kwT[:], FMd[FM_NKW:FM_NKW + 64, :], [FMd], [kwT])
                    vN = ph.sb("vN", [128, 32, 130], BF16)
                    kb.dma(vN[:], VTM[:, 390:520].rearrange("(c p) f -> p c f", p=128), [VTM], [vN])
                    cmpmask = ph.sb("cmpmask", [128, 2, S], BF16)
                    kb.dma(cmpmask[:], cin["c_cmpmask"], [], [cmpmask])
                    addc = ph.sb("addc", [128, 32, 64], F32)
                    multm = ph.sb("multm", [128, 32, 64], F32)
                    tri = ph.sb("tri", [128, 4, 128], BF16)
                    tri2 = ph.sb("tri2", [128, 4, 128], BF16)
                    gain_n = ph.sb("gain_n", [128, 256], F32)
                    for t_, n_ in ((addc, "c_addc"), (multm, "c_mult"), (tri, "c_tri"), (tri2, "c_tri2")):
                        kb.dma(t_[:], cin[n_], [], [t_])
                    kb.dma(gain_n[:], gains[L, :, 768:1024], [], [gain_n])
                    qAs = [ph.sb("qA%d" % i, [128, 4, 128], BF16) for i in range(2)]
                    Et = ph.sb("Et", [128, 2, 512], BF16)
                    pts = [ph.sb("npt%d" % i, [128, 512], BF16) for i in range(2)]
                    rzc = ph.sb("rzc", [128, 4], F32)
                    imp = ph.sb("imp", [128, 64], F32)
                    score = ph.sb("score", [128, 64], F32)
                    sc2 = ph.sb("sc2", [128, 64], F32)
                    m8 = ph.sb("m8", [128, 8], F32)
                    m8b = ph.sb("m8b", [128, 8], F32)
                    negm = ph.sb("negm", [128, 64], BF16)
                    zall = ph.sb("zall", [128, 4, 3], F32)
                    coef = ph.sb("coef", [128, 4, 3], F32)
                    on = ph.sb("non", [128, 4, 64], F32)
                    osq = ph.sb("nosq", [128, 4, 64], F32)
                    ss = ph.sb("nss", [128, 4], F32)
                    yns = [ph.sb("yn%d" % i, [128, 4, 64], BF16) for i in range(2)]
                    tri_2d = tri[:].rearrange("p h t -> p (h t)")
                    tri2_2d = tri2[:].rearrange("p h t -> p (h t)")

                    def load_qa(b):
                        qa = qAs[b % 2]
                        kb.dma(qa[0:64, :, :], FMd[FM_NQ:FM_NQ + 256, b * 128:(b + 1) * 128].rearrange("(h d) t -> d h t", d=64), [FMd], [qa])

                    on_cs = [ph.sb("onc%d" % i, [128, 4, 64], F32) for i in range(2)]
                    gco = ph.sb("gco", [128, 4], F32)
                    ots_s = ph.sb("ots_s", [65, 512], F32)
                    ots_w = ph.sb("ots_w", [65, 512], F32)
                    zc4 = ph.sb("zc4", [128, 4], F32)
                    pos_ = ps[5]
                    pow_ = ps[0]
                    cnt_box = [0]

                    def oc_views():
                        return [ps[3 + k][:, 0:258].rearrange("p (h f) -> p h f", f=129) for k in range(2)]

                    def cmp_pe1(b):
                        qa = qAs[b % 2]
                        q2 = qa[:].rearrange("p h t -> p (h t)")
                        ncn = 2 if b >= 16 else 1
                        for n_ in range(ncn):
                            pse = ps[1 + n_]
                            kb.op(pe, [kcT, qa], [pse], lambda e: e.matmul(pse[:, :], lhsT=kcT[0:64, n_ * 128:(n_ + 1) * 128], rhs=q2[0:64, :], start=True, stop=True))
                            kb.op(act, [pse], [Et], lambda e: e.activation(out=Et[:, n_, :], in_=pse[:, :], func=AF.Exp))
                            kb.op(pool, [Et, cmpmask], [Et], lambda e: e.tensor_tensor(out=Et[:, n_, :].rearrange("p (h t) -> p h t", t=128), in0=Et[:, n_, :].rearrange("p (h t) -> p h t", t=128), in1=cmpmask[:, n_, b * 128:(b + 1) * 128].unsqueeze(1).to_broadcast([128, 4, 128]), op=ALU.mult))

                    def cmp_pe2(b):
                        ncn = 2 if b >= 16 else 1
                        for h in range(4):
                            bank = ps[3 + h // 2]
                            off = (h % 2) * 129
                            for n_ in range(ncn):
                                kb.op(pe, [Et, VCM], [bank], lambda e: e.matmul(bank[:, off:off + 129], lhsT=Et[:, n_, h * 128:(h + 1) * 128], rhs=VCM[:, n_, :], start=(n_ == 0), stop=(n_ == ncn - 1)))

                    def chain(b):
                        oc = oc_views()
                        for k in range(2):
                            kb.op(dve, [ps[3 + k]], [zc4], lambda e: e.tensor_scalar(out=zc4[:, 2 * k:2 * k + 2], in0=oc[k][:, :, 128], scalar1=1e-30, scalar2=None, op0=ALU.add))
                        kb.op(dve, [zc4], [rzc], lambda e: e.reciprocal(out=rzc[:], in_=zc4[:]))
                        for h in range(4):
                            src = oc[h // 2][:, h % 2, 64:128]
                            if h == 0:
                                kb.op(dve, [ps[3], rzc], [imp], lambda e: e.tensor_scalar(out=imp[:], in0=src, scalar1=rzc[:, 0:1], scalar2=None, op0=ALU.mult))
                            else:
                                kb.op(dve, [ps[3 + h // 2], rzc, imp], [imp], lambda e: e.scalar_tensor_tensor(out=imp[:], in0=src, scalar=rzc[:, h:h + 1], in1=imp[:], op0=ALU.mult, op1=ALU.add))
                        kb.op(dve, [imp, multm], [score], lambda e: e.tensor_tensor(out=score[:], in0=imp[:], in1=multm[:, b, :], op=ALU.mult))
                        kb.op(dve, [score, addc], [score], lambda e: e.tensor_tensor(out=score[:], in0=score[:], in1=addc[:, b, :], op=ALU.add))
                        kb.op(dve, [score], [m8], lambda e: e.max(out=m8[:], in_=score[:]))
                        kb.op(dve, [score, m8], [sc2], lambda e: e.match_replace(out=sc2[:], in_to_replace=m8[:], in_values=score[:], imm_value=-1e9))
                        kb.op(dve, [sc2], [m8b], lambda e: e.max(out=m8b[:], in_=sc2[:]))
                        kb.op(dve, [score, m8b], [negm], lambda e: e.tensor_scalar(out=negm[:], in0=score[:], scalar1=m8b[:, 7:8], scalar2=NEG, op0=ALU.is_lt, op1=ALU.mult))
                        kb.op(dve, [rzc, gates_sb], [gco], lambda e: e.tensor_tensor(out=gco[:], in0=rzc[:], in1=gates_sb[:, b, :].rearrange("p (h g) -> p h g", g=3)[:, :, 0], op=ALU.mult))
                        onc = on_cs[b % 2]
                        for k in range(2):
                            kb.op(dve, [ps[3 + k], gco], [onc], lambda e: e.tensor_tensor(out=onc[:, 2 * k:2 * k + 2, :], in0=oc[k][:, :, 0:64], in1=gco[:, 2 * k:2 * k + 2].unsqueeze(2).to_broadcast([128, 2, 64]), op=ALU.mult))

                    def tr_copy(b):
                        qa = qAs[b % 2]
                        kb.op(pe, [negm, identb], [psb], lambda e: e.transpose(psb[0:64, 0:128], negm[:, :], identb[:, :]))
                        kb.op(act, [psb], [qa], lambda e: e.activation(out=qa[64:128, :, :], in_=psb[0:64, 0:128].unsqueeze(1).to_broadcast([64, 4, 128]), func=AF.Copy))

                    def qk_item(b, it):
                        qa = qAs[b % 2]
                        q2 = qa[:].rearrange("p h t -> p (h t)")
                        kind, c = it["kind"], it["c"]
                        sb_ = ps[1 + (it["slot"] % 2)]
                        if kind == "w":
                            edge = (c == b) or (c == b - 4)
                            kb.op(pe, [kwT, qa], [sb_], lambda e: e.matmul(sb_[:, :], lhsT=kwT[0:64, c * 128:(c + 1) * 128], rhs=q2[0:64, :], start=True, stop=not edge))
                            if edge:
                                mt, m2d = (tri, tri_2d) if c == b else (tri2, tri2_2d)
                                kb.op(pe, [identb, mt], [sb_], lambda e: e.matmul(sb_[:, :], lhsT=identb[:, :], rhs=m2d, start=False, stop=True))
                        else:
                            kb.op(pe, [ksA, qa], [sb_], lambda e: e.matmul(sb_[:, :], lhsT=ksA[:, c * 128:(c + 1) * 128], rhs=q2[:, :], start=True, stop=(c != b)))
                            if c == b:
                                kb.op(pe, [identb, tri], [sb_], lambda e: e.matmul(sb_[:, :], lhsT=identb[:, :], rhs=tri_2d, start=False, stop=True))

                    def ep_item(b, it):
                        kind, c = it["kind"], it["c"]
                        c_lo = max(0, b - 4)
                        sb_ = ps[1 + (it["slot"] % 2)]
                        pt = pts[it["slot"] % 2]
                        kb.op(act, [sb_], [pt], lambda e: e.activation(out=pt[:], in_=sb_[:, :], func=AF.Exp))
                        if kind == "w":
                            kb.op(pe, [pt, vN], [pow_], lambda e: e.matmul(pow_[0:65, :], lhsT=vN[:, c, 65:130], rhs=pt[:, :], start=(c == c_lo), stop=(c == b)))
                        else:
                            kb.op(pe, [pt, vN], [pos_], lambda e: e.matmul(pos_[0:65, :], lhsT=vN[:, c, 0:65], rhs=pt[:, :], start=(c == 0), stop=(c == b)))

                    def run_items(b, lst):
                        for it in lst:
                            it["slot"] = cnt_box[0]
                            cnt_box[0] += 1
                        if lst:
                            qk_item(b, lst[0])
                        for k in range(len(lst)):
                            if k + 1 < len(lst):
                                qk_item(b, lst[k + 1])
                            ep_item(b, lst[k])

                    def combine(b):
                        kb.op(act, [pos_], [ots_s], lambda e: e.activation(out=ots_s[:], in_=pos_[0:65, :], func=AF.Copy))
                        kb.op(dve, [pow_], [ots_w], lambda e: e.tensor_copy(out=ots_w[:], in_=pow_[0:65, :]))
                        for h in range(4):
                            kb.op(pe, [ots_s, identf], [pos_], lambda e: e.transpose(pos_[:, h * 65:(h + 1) * 65], ots_s[0:65, h * 128:(h + 1) * 128], identf[0:65, 0:65]))
                        for h in range(4):
                            kb.op(pe, [ots_w, identf], [pow_], lambda e: e.transpose(pow_[:, h * 65:(h + 1) * 65], ots_w[0:65, h * 128:(h + 1) * 128], identf[0:65, 0:65]))
                        os3 = pos_[:, 0:260].rearrange("p (h f) -> p h f", f=65)
                        ow3 = pow_[:, 0:260].rearrange("p (h f) -> p h f", f=65)
                        g3 = gates_sb[:, b, :].rearrange("p (h g) -> p h g", g=3)
                        kb.op(dve, [pos_], [zall], lambda e: e.tensor_copy(out=zall[:, :, 1], in_=os3[:, :, 64]))
                        kb.op(dve, [pow_], [zall], lambda e: e.tensor_copy(out=zall[:, :, 2], in_=ow3[:, :, 64]))
                        kb.op(dve, [zall], [coef], lambda e: e.reciprocal(out=coef[:, :, 1:3], in_=zall[:, :, 1:3]))
                        kb.op(dve, [coef, gates_sb], [coef], lambda e: e.tensor_tensor(out=coef[:, :, 1:3], in0=coef[:, :, 1:3], in1=g3[:, :, 1:3], op=ALU.mult))
                        kb.op(dve, [pos_, coef], [on], lambda e: e.tensor_tensor(out=on[:], in0=os3[:, :, 0:64], in1=coef[:, :, 1:2].to_broadcast([128, 4, 64]), op=ALU.mult))
                        kb.op(dve, [pow_, coef], [osq], lambda e: e.tensor_tensor(out=osq[:], in0=ow3[:, :, 0:64], in1=coef[:, :, 2:3].to_broadcast([128, 4, 64]), op=ALU.mult))
                        kb.op(pool, [on, osq], [on], lambda e: e.tensor_tensor(out=on[:], in0=on[:], in1=osq[:], op=ALU.add))
                        onc = on_cs[b % 2]
                        kb.op(pool, [on, onc], [on], lambda e: e.tensor_tensor(out=on[:], in0=on[:], in1=onc[:], op=ALU.add))
                        yn = yns[b % 2]
                        norm_gain(4, on, osq, ss, gain_n[:].rearrange("p (h d) -> p h d", d=64), yn, yn[:], gain_n)
                        kb.dma(Yd[b * 128:(b + 1) * 128, 768:1024], yn[:].rearrange("p h d -> p (h d)"), [yn], [Yd])

                    load_qa(0)
                    cmp_pe1(0)
                    cmp_pe2(0)
                    chain(0)
                    for b in range(32):
                        if b + 1 < 32:
                            load_qa(b + 1)
                        run_items(b, [{"kind": "w", "c": c} for c in range(max(0, b - 4), b + 1)])
                        tr_copy(b)
                        sel = [{"kind": "s", "c": c} for c in range(b + 1)]
                        half = len(sel) // 2
                        run_items(b, sel[:half])
                        if b + 1 < 32:
                            cmp_pe1(b + 1)
                        run_items(b, sel[half:])
                        if b + 1 < 32:
                            cmp_pe2(b + 1)
                            chain(b + 1)
                        combine(b)
                if debug == "N":
                    break

            with kb.phase() as ph:
                woT = ph.sb("woT", [128, 8, 1024], BF16)
                stg = [ph.sb("wos%d" % i, [128, 1024], F32) for i in range(2)]
                load_cast(ph, woT, lambda k: woT[:, k, :], lambda k: w_out[L, k * 128:(k + 1) * 128, :], 128, 1024, 8, stg, [act, dve])
                oxts = [ph.sb("oxt%d" % i, [128, 8, 512], F32) for i in range(2)]
                oYs = [ph.sb("oY%d" % i, [128, 4, 1024], BF16) for i in range(2)]
                yT = ph.sb("oyT", [128, 8, 512], BF16)

                def load_o(Tt):
                    kb.dma(oxts[Tt % 2][:], x_cur[:, :].rearrange("(c p) t -> p c t", p=128)[:, :, Tt * 512:(Tt + 1) * 512], [x_cur], [oxts[Tt % 2]])
                    kb.dma(oYs[Tt % 2][:], Yd[Tt * 512:(Tt + 1) * 512, :].rearrange("(s p) f -> p s f", p=128), [Yd], [oYs[Tt % 2]])

                load_o(0)
                for Tt in range(8):
                    xt, Yt = oxts[Tt % 2], oYs[Tt % 2]
                    if Tt + 1 < 8:
                        load_o(Tt + 1)
                    for sub in range(4):
                        pb = psb if sub % 2 == 0 else psb2
                        for kc in range(8):
                            kb.op(pe, [Yt, identb], [pb], lambda e: e.transpose(pb[:, kc * 128:(kc + 1) * 128], Yt[:, sub, kc * 128:(kc + 1) * 128], identb[:, :]))
                        src = pb[:, 0:1024].rearrange("p (k t) -> p k t", t=128)
                        if sub % 2 == 0:
                            kb.op(act, [pb], [yT], lambda e: e.activation(out=yT[:, :, sub * 128:(sub + 1) * 128], in_=src, func=AF.Copy))
                        else:
                            kb.op(dve, [pb], [yT], lambda e: e.tensor_copy(out=yT[:, :, sub * 128:(sub + 1) * 128], in_=src))
                    for oc in range(8):
                        bank = ps[oc % 4]
                        for kc in range(8):
                            kb.op(pe, [woT, yT], [bank], lambda e: e.matmul(bank[:, :], lhsT=woT[:, kc, oc * 128:(oc + 1) * 128], rhs=yT[:, kc, :], start=(kc == 0), stop=(kc == 7)))
                        kb.op(dve, [bank, xt], [xt], lambda e: e.tensor_tensor(out=xt[:, oc, :], in0=bank[:, :], in1=xt[:, oc, :], op=ALU.add))
                    kb.dma(x1[:, :].rearrange("(c p) t -> p c t", p=128)[:, :, Tt * 512:(Tt + 1) * 512], xt[:], [xt], [x1])
            if debug == "O":
                break

            last = (L == n_layers - 1)
            with kb.phase() as ph:
                w1T = ph.sb("w1T", [128, 8, 4096], BF16)
                w2T = ph.sb("w2T", [128, 32, 1024], BF16)
                stg = [ph.sb("ms%d" % i, [128, 1024], F32) for i in range(4)]
                load_cast(ph, w1T, lambda k: w1T[:, k // 4, (k % 4) * 1024:(k % 4 + 1) * 1024], lambda k: w_m1[L, (k // 4) * 128:(k // 4 + 1) * 128, (k % 4) * 1024:(k % 4 + 1) * 1024], 128, 1024, 32, stg, [act, dve, act, dve, pool, act, dve])
                load_cast(ph, w2T, lambda k: w2T[:, k, :], lambda k: w_m2[L, k * 128:(k + 1) * 128, :], 128, 1024, 32, stg, [act, dve, act, dve, pool, act, dve])
                g2 = ph.sb("g2", [128, 8], F32)
                kb.dma(g2[:], g_mlp[L], [], [g2])
                gf = ph.sb("gf", [128, 8], F32)
                kb.dma(gf[:], g_fin, [], [gf])
                mxts = [ph.sb("mxt%d" % i, [128, 8, 256], F32) for i in range(2)]
                msq = ph.sb("msq", [128, 8, 256], BF16)
                mhT = ph.sb("mhT", [128, 8, 256], BF16)
                mrstd = ph.sb("mrstd", [128, 256], F32)
                hidT = ph.sb("hidT", [128, 32, 256], BF16)
                rts = [ph.sb("mrt%d" % i, [128, 256], F32) for i in range(3)]
                ofin = ph.sb("ofin", [128, 8, 256], F32) if (last and final_norm) else None

                def load_m(Tt):
                    kb.dma(mxts[Tt % 2][:], x1[:, :].rearrange("(c p) t -> p c t", p=128)[:, :, Tt * 256:(Tt + 1) * 256], [x1], [mxts[Tt % 2]])

                load_m(0)
                for Tt in range(16):
                    xt = mxts[Tt % 2]
                    if Tt + 1 < 16:
                        load_m(Tt + 1)
                    rmsnorm_tile(ph, xt, msq, mhT, g2, mrstd, 256, ps[0])
                    for f in range(32):
                        bank = ps[1 + f % 3]
                        for kc in range(8):
                            kb.op(pe, [w1T, mhT], [bank], lambda e: e.matmul(bank[:, 0:256], lhsT=w1T[:, kc, f * 128:(f + 1) * 128], rhs=mhT[:, kc, :], start=(kc == 0), stop=(kc == 7)))
                        rt = rts[f % 3]
                        kb.op(act, [bank], [rt], lambda e: e.activation(out=rt[:], in_=bank[:, 0:256], func=AF.Relu))
                        kb.op(dve if f % 2 == 0 else pool, [rt], [hidT], lambda e: e.tensor_tensor(out=hidT[:, f, :], in0=rt[:], in1=rt[:], op=ALU.mult))
                    for oc in range(8):
                        bank = ps[4 + oc % 2]
                        for f in range(32):
                            kb.op(pe, [w2T, hidT], [bank], lambda e: e.matmul(bank[:, 0:256], lhsT=w2T[:, f, oc * 128:(oc + 1) * 128], rhs=hidT[:, f, :], start=(f == 0), stop=(f == 31)))
                        kb.op(dve, [bank, xt], [xt], lambda e: e.tensor_tensor(out=xt[:, oc, :], in0=bank[:, 0:256], in1=xt[:, oc, :], op=ALU.add))
                    if last and final_norm:
                        rmsnorm_tile(ph, xt, msq, ofin, gf, mrstd, 256, ps[0])
                        kb.dma(out_T[:, :].rearrange("(c p) t -> p c t", p=128)[:, :, Tt * 256:(Tt + 1) * 256], ofin[:], [ofin], [out_T])
                    else:
                        dstx = out_T if last else xA
                        kb.dma(dstx[:, :].rearrange("(c p) t -> p c t", p=128)[:, :, Tt * 256:(Tt + 1) * 256], xt[:], [xt], [dstx])
            x_cur = xA
    return nc


def _prep_inputs(inputs):
    x = np.asarray(inputs["x"], np.float32)
    w_in = np.asarray(inputs["w_in"], np.float32)
    sizes = (384, 384, 384, 384, 384, 384, 384, 6, 256, 64, 64, 64, 64, 64, 64, 12)
    offs = np.concatenate([[0], np.cumsum(sizes)])
    names = ["rq", "rk", "rv", "rg", "fq", "fk", "fv", "ff", "nq", "nkc", "nvc", "nks", "nvs", "nkw", "nvw", "gate"]
    sl = {n: np.arange(offs[i], offs[i + 1]) for i, n in enumerate(names)}
    order = ["rq", "rk", "rv", "nvs", "nvw", "rg", "gate", "fv", "fq", "fk", "nq", "nkc", "nvc", "nks", "nkw", "ff"]
    perm = np.concatenate([sl[n] for n in order])
    common = {
        "w_in": np.ascontiguousarray(w_in[:, :, perm]),
        "w_out": np.asarray(inputs["w_out"], np.float32),
        "w_m1": np.asarray(inputs["w_mlp_in"], np.float32),
        "w_m2": np.asarray(inputs["w_mlp_out"], np.float32),
        "w1k": np.asarray(inputs["nsa_cmp_w1_k"], np.float32),
        "w1v": np.asarray(inputs["nsa_cmp_w1_v"], np.float32),
        "w2k": np.asarray(inputs["nsa_cmp_w2_k"], np.float32),
        "w2v": np.asarray(inputs["nsa_cmp_w2_v"], np.float32),
        "g_attn": np.ascontiguousarray(np.asarray(inputs["norm_attn"], np.float32).reshape(NL, 8, 128).transpose(0, 2, 1)),
        "g_mlp": np.ascontiguousarray(np.asarray(inputs["norm_mlp"], np.float32).reshape(NL, 8, 128).transpose(0, 2, 1)),
        "g_fin": np.ascontiguousarray(np.asarray(inputs["norm_final"], np.float32).reshape(8, 128).T),
        "gains": np.ascontiguousarray(np.broadcast_to(np.concatenate([np.asarray(inputs["ret_norm_gain"], np.float32), np.asarray(inputs["fox_norm_gain"], np.float32), np.asarray(inputs["nsa_norm_gain"], np.float32)], axis=1)[:, None, :], (NL, 128, 1024))),
        "fbias": np.asarray(inputs["fox_forget_bias"], np.float32).reshape(NL, 6, 1),
        "posk": np.ascontiguousarray(np.asarray(inputs["nsa_cmp_pos_k"], np.float32).transpose(0, 2, 1)),
        "posv": np.ascontiguousarray(np.asarray(inputs["nsa_cmp_pos_v"], np.float32).transpose(0, 2, 1)),
    }
    return x, common


def kernel(**inputs):
    x, common = _prep_inputs(inputs)
    nc = build()
    common.update(CONST_SPECS)
    in_maps = []
    for b in range(8):
        m = dict(common)
        m["xT"] = np.ascontiguousarray(x[b].T)
        in_maps.append(m)
    res = run_bass_kernel_spmd(nc, in_maps, core_ids=list(range(8)))
    out = np.stack([np.ascontiguousarray(r["outT"].T) for r in res.results], axis=0)
    return out.astype(np.float32)
```

```python
import contextlib
import numpy as np
import ml_dtypes
import concourse.bass as bass
import concourse.mybir as mybir
from concourse.bass_utils import run_bass_kernel_spmd

F32 = mybir.dt.float32
BF16 = mybir.dt.bfloat16
AF = mybir.ActivationFunctionType
ALU = mybir.AluOpType
AX = mybir.AxisListType

S = 4096
D = 1024
NL = 4
EPS = 1e-6
NEG = -30000.0
TM_RQ, TM_RK, TM_RV, TM_NVS, TM_NVW, TM_RG, TM_GATE, TM_FV, TM_END = 0, 384, 768, 1152, 1216, 1280, 1664, 1676, 2060
FM0 = TM_END
FM_FQ, FM_FK, FM_NQ, FM_NKC, FM_NVC, FM_NKS, FM_NKW, FM_FF, FM_END = 0, 384, 768, 1024, 1088, 1152, 1216, 1280, 1286
WCOLS = TM_END + FM_END


class Tok:
    def __init__(self, sem, inc):
        self.sem, self.inc, self.count = sem, inc, 0


class Buf:
    def __init__(self, name):
        self.name, self.w, self.r, self.dtok, self.excl = name, {}, {}, None, False


class T:
    def __init__(self, t, name):
        self.t, self.b = t, Buf(name)

    def __getitem__(self, k):
        return self.t[k]


class Eng:
    def __init__(self, name, eng, tok):
        self.name, self.eng, self.tok, self.seen = name, eng, tok, {}


def _b(x):
    return x.b if isinstance(x, T) else x


class KB:
    def __init__(self, nc, es):
        self.nc, self.es = nc, es
        self.free_dtoks = []
        self.all_toks = []
        mk = lambda n: self._newtok(n, 1)
        self.pe = Eng("pe", nc.tensor, mk("s_pe"))
        self.act = Eng("act", nc.scalar, mk("s_act"))
        self.dve = Eng("dve", nc.vector, mk("s_dve"))
        self.pool = Eng("pool", nc.gpsimd, mk("s_pool"))
        self.sp = Eng("sp", nc.sync, None)
        self.engs = [self.pe, self.act, self.dve, self.pool, self.sp]
        self.nsem = 4
        self.nops = 0
        import os
        self.limit = int(os.environ.get("KLIMIT", "1000000000"))

    def _newtok(self, name, inc):
        sem = self.es.enter_context(self.nc.semaphore(name))
        t = Tok(sem, inc)
        self.all_toks.append(t)
        return t

    def _sync(self, E, reads, writes, is_dma=False):
        deps = {}

        def add(tok, c, kind):
            if (not is_dma) and tok is E.tok:
                if kind == "war" or E.name == "pe":
                    return
            if deps.get(tok, 0) < c:
                deps[tok] = c

        for x in reads:
            b = _b(x)
            for tok, c in b.w.items():
                add(tok, c, "raw")
            if b.excl:
                for tok, c in b.r.items():
                    if tok is not E.tok:
                        add(tok, c, "rar")
        for x in writes:
            b = _b(x)
            for tok, c in b.w.items():
                add(tok, c, "waw")
            for tok, c in b.r.items():
                add(tok, c, "war")
        for tok, c in deps.items():
            if E.seen.get(tok, 0) < c:
                E.eng.wait_ge(tok.sem, c * tok.inc)
                E.seen[tok] = c

    def _mark(self, tok, reads, writes):
        for x in reads:
            _b(x).r[tok] = tok.count
        for x in writes:
            _b(x).w[tok] = tok.count

    def op(self, E, reads, writes, fn):
        self.nops += 1
        if self.nops > self.limit:
            return None
        self._sync(E, reads, writes)
        ins = fn(E.eng)
        E.tok.count += 1
        ins.then_inc(E.tok.sem, 1)
        E.seen[E.tok] = max(E.seen.get(E.tok, 0), 0)
        self._mark(E.tok, reads, writes)
        return ins

    def dma(self, out_ap, in_ap, reads, writes, Q=None, **kw):
        self.nops += 1
        if self.nops > self.limit:
            return None
        Q = Q or self.sp
        dst = _b(writes[0])
        if dst.dtok is None:
            if self.free_dtoks:
                dst.dtok = self.free_dtoks.pop()
            else:
                self.nsem += 1
                dst.dtok = self._newtok("s_d%d" % self.nsem, 16)
        self._sync(Q, reads, writes, is_dma=True)
        ins = Q.eng.dma_start(out=out_ap, in_=in_ap, **kw)
        tok = dst.dtok
        tok.count += 1
        ins.then_inc(tok.sem, 16)
        self._mark(tok, reads, writes)

    def barrier(self):
        for E in self.engs:
            for tok in self.all_toks:
                if tok.count > 0 and E.seen.get(tok, 0) < tok.count and not (tok is E.tok and E.name == "pe" and False):
                    E.eng.wait_ge(tok.sem, tok.count * tok.inc)
                    E.seen[tok] = tok.count

    @contextlib.contextmanager
    def phase(self):
        ph = Phase(self)
        with ph.es:
            yield ph
            self.barrier()
        for t in ph.tensors:
            if t.b.dtok is not None:
                self.free_dtoks.append(t.b.dtok)
                t.b.dtok = None


class Phase:
    def __init__(self, kb):
        self.kb, self.es, self.tensors = kb, contextlib.ExitStack(), []

    def sb(self, name, shape, dt):
        self.kb.uid = getattr(self.kb, "uid", 0) + 1
        name = "%s_u%d" % (name, self.kb.uid)
        t = T(self.es.enter_context(self.kb.nc.sbuf_tensor(name, list(shape), dt)), name)
        self.tensors.append(t)
        return t


def make_consts():
    c = {}
    bf = ml_dtypes.bfloat16
    inv = (1.0 / (np.float32(10000.0) ** (np.arange(32, dtype=np.float32) / np.float32(32)))).astype(np.float32)
    ang = (np.arange(S, dtype=np.float32)[:, None] * inv[None, :]).astype(np.float32).astype(np.float64)
    tm = lambda a: np.ascontiguousarray(a.reshape(32, 128, -1).transpose(1, 0, 2))
    c["c_cos"] = tm(np.cos(ang).astype(np.float32))
    c["c_sin"] = tm(np.sin(ang).astype(np.float32))
    lg = np.log(1.0 - 2.0 ** (-5.0 - np.arange(6, dtype=np.float64)))
    idx = np.arange(128, dtype=np.float64)
    dec = np.zeros((128, 6, 128), np.float64)
    for h in range(6):
        diff = idx[None, :] - idx[:, None]
        dec[:, h, :] = np.where(diff >= 0, np.exp(lg[h] * np.maximum(diff, 0)), 0.0) * 0.125
    c["c_dec"] = dec.astype(np.float32)
    dq = np.zeros((64, 6, 128), np.float64)
    for h in range(6):
        dq[:, h, :] = np.exp(lg[h] * (idx + 1.0))[None, :]
    c["c_dq"] = dq.astype(np.float32)
    c["c_dk"] = (np.exp(lg[None, :] * (127.0 - idx[:, None])) * 0.125).astype(np.float32)
    c["c_identb"] = np.eye(128, dtype=np.float32).astype(bf)
    c["c_identf"] = np.eye(128, dtype=np.float32)
    sel = np.zeros((128, 128), np.float32)
    sel[127, :] = 1.0
    c["c_sel127"] = sel
    c["c_onesn"] = np.full((128, 128), 1.0 / 1024.0, np.float32).astype(bf)
    s_ = np.arange(128)[:, None, None]
    i_ = np.arange(4)[None, :, None]
    t_ = np.arange(512)[None, None, :]
    c["c_fmask"] = np.where(128 * i_ + s_ <= t_, 0.0, NEG).astype(np.float32).astype(bf)
    tl = np.arange(128)[None, None, :]
    c["c_tri"] = np.broadcast_to(np.where(s_ <= tl, 0.0, NEG), (128, 4, 128)).astype(np.float32).astype(bf)
    c["c_tri2"] = np.broadcast_to(np.where(s_ > tl, 0.0, NEG), (128, 4, 128)).astype(np.float32).astype(bf)
    tok = np.arange(S)
    c["c_expand"] = (tok[None, :] // 64 == np.arange(64)[:, None]).astype(np.float32).astype(bf)
    n_cmp = 255
    cs = np.arange(n_cmp) * 16
    ss = np.arange(64) * 64
    ov = np.clip(np.minimum(cs[:, None] + 32, ss[None, :] + 64) - np.maximum(cs[:, None], ss[None, :]), 0, None)
    M = np.zeros((256, 64), np.float32)
    M[:255] = ov / 32.0
    c["c_cmp2sel"] = np.ascontiguousarray(M.reshape(2, 128, 64).transpose(1, 0, 2)).astype(bf)
    n_all = np.arange(256)
    cm = ((16 * n_all[:, None] + 31 <= tok[None, :]) & (n_all[:, None] < 255)).astype(np.float32)
    c["c_cmpmask"] = np.ascontiguousarray(cm.reshape(2, 128, S).transpose(1, 0, 2)).astype(bf)
    addc = np.zeros((S, 64), np.float32)
    mult = np.ones((S, 64), np.float32)
    sid = np.arange(64)
    for t in range(S):
        cur = t // 64
        inv_ = sid > cur
        addc[t, inv_] = -1.0 - 0.01 * sid[inv_]
        mult[t, inv_] = 0.0
        for s_f, val in ((cur - 1, 3e4), (cur, 2e4), (0, 1e4)):
            if s_f >= 0:
                addc[t, s_f] = val
                mult[t, s_f] = 0.0
    c["c_addc"] = tm(addc)
    c["c_mult"] = tm(mult)
    return c


CONST_SPECS = None


def build(n_layers=NL, debug=False, final_norm=True):
    global CONST_SPECS
    consts = make_consts()
    CONST_SPECS = consts
    nc = bass.Bass("TRN2", target_bir_lowering=False)
    es = contextlib.ExitStack()
    dbg_kind = "ExternalOutput" if debug else "Internal"

    def din(name, shape, dt=F32):
        return nc.dram_tensor(name, list(shape), dt, kind="ExternalInput").ap()

    def dscr(name, shape, dt, kind=None):
        return T(nc.dram_tensor(name, list(shape), dt, kind=kind or dbg_kind).ap(), name)

    xT_in = T(din("xT", [D, S]), "xT")
    w_in = din("w_in", [NL, D, WCOLS])
    w_out = din("w_out", [NL, D, D])
    w_m1 = din("w_m1", [NL, D, 4 * D])
    w_m2 = din("w_m2", [NL, 4 * D, D])
    w1k = din("w1k", [NL, 2048, 256])
    w1v = din("w1v", [NL, 2048, 256])
    w2k = din("w2k", [NL, 256, 64])
    w2v = din("w2v", [NL, 256, 64])
    g_attn = din("g_attn", [NL, 128, 8])
    g_mlp = din("g_mlp", [NL, 128, 8])
    g_fin = din("g_fin", [128, 8])
    gains = din("gains", [NL, 128, 1024])
    fbias = din("fbias", [NL, 6, 1])
    posk = din("posk", [NL, 64, 32])
    posv = din("posv", [NL, 64, 32])
    cin = {k: din(k, v.shape, BF16 if v.dtype == ml_dtypes.bfloat16 else F32) for k, v in consts.items()}

    out_T = T(nc.dram_tensor("outT", [D, S], F32, kind="ExternalOutput").ap(), "outT")
    xA = dscr("xA", [D, S], F32)
    x1 = dscr("x1", [D, S], F32)
    FMd = dscr("FMd", [1280, S], BF16)
    VTM = dscr("VTM", [S, 520], BF16)
    Yd = dscr("Yd", [S, 1024], BF16)
    DQd = dscr("DQd", [2, 6, S], BF16)

    if debug:
        dbg_lf = dscr("dbg_lf", [6, S], F32)
        dbg_gates = dscr("dbg_gates", [128, 32, 12], F32)

    with es:
        kb = KB(nc, es)
        pe, act, dve, pool = kb.pe, kb.act, kb.dve, kb.pool
        ps = [T(es.enter_context(nc.psum_tensor("ps%d" % i, [128, 512], F32)), "ps%d" % i) for i in range(6)]
        psb = T(es.enter_context(nc.psum_tensor("psb", [128, 1024], BF16)), "psb")
        ps6 = T(es.enter_context(nc.psum_tensor("ps6", [128, 512], F32)), "ps6")
        psb2 = T(ps6[:, :].bitcast(BF16), "psb2")
        psb2.b = ps6.b
        for t_ in ps + [psb, ps6]:
            t_.b.excl = True

        pers = Phase(kb)
        es.enter_context(pers.es)
        identb = pers.sb("identb", [128, 128], BF16)
        identf = pers.sb("identf", [128, 128], F32)
        onesn = pers.sb("onesn", [128, 128], BF16)
        for tname, t in (("c_identb", identb), ("c_identf", identf), ("c_onesn", onesn)):
            kb.dma(t[:], cin[tname][:, :], [], [t])

        def load_cast(ph, dst, dst_ap_fn, src_ap_fn, nparts, ncols, nk, stage, engines):
            for k in range(nk):
                st = stage[k % len(stage)]
                kb.dma(st[0:nparts, 0:ncols], src_ap_fn(k), [], [st])
                E = engines[k % len(engines)]
                if E is act:
                    kb.op(E, [st], [dst], lambda e: e.activation(out=dst_ap_fn(k), in_=st[0:nparts, 0:ncols], func=AF.Copy))
                else:
                    kb.op(E, [st], [dst], lambda e: e.tensor_copy(out=dst_ap_fn(k), in_=st[0:nparts, 0:ncols]))

        epsc = pers.sb("epsc", [128, 1], F32)
        kb.op(dve, [], [epsc], lambda e: e.memset(epsc[:], EPS))

        def rsqrt(oT, o_ap, iT, i_ap, scale):
            npart = o_ap.shape[0]
            kb.op(act, [iT, epsc], [oT], lambda e: e.activation(out=o_ap, in_=i_ap, func=AF.Ln, bias=epsc[0:npart, :], scale=scale))
            kb.op(act, [oT], [oT], lambda e: e.activation(out=o_ap, in_=o_ap, func=AF.Exp, scale=-0.5))

        def head_finish(o3, nb, on, osq, rinv, ss, gain_ap, yT, y_ap, srcT, gT):
            kb.op(dve, [srcT], [rinv], lambda e: e.reciprocal(out=rinv[:, 0:nb], in_=o3[:, :, 64]))
            kb.op(dve, [srcT, rinv], [on], lambda e: e.tensor_tensor(out=on[:, 0:nb, :], in0=o3[:, :, 0:64], in1=rinv[:, 0:nb].unsqueeze(2).to_broadcast([128, nb, 64]), op=ALU.mult))
            norm_gain(nb, on, osq, ss, gain_ap, yT, y_ap, gT)

        def norm_gain(nb, on, osq, ss, gain_ap, yT, y_ap, gT):
            kb.op(pool, [on], [osq], lambda e: e.tensor_tensor(out=osq[:, 0:nb, :], in0=on[:, 0:nb, :], in1=on[:, 0:nb, :], op=ALU.mult))
            kb.op(dve, [osq], [ss], lambda e: e.tensor_reduce(out=ss[:, 0:nb], in_=osq[:, 0:nb, :], axis=AX.X, op=ALU.add))
            rsqrt(ss, ss[:, 0:nb], ss, ss[:, 0:nb], 1.0 / 64.0)
            kb.op(dve, [on, ss], [on], lambda e: e.tensor_tensor(out=on[:, 0:nb, :], in0=on[:, 0:nb, :], in1=ss[:, 0:nb].unsqueeze(2).to_broadcast([128, nb, 64]), op=ALU.mult))
            kb.op(pool, [on, gT], [yT], lambda e: e.tensor_tensor(out=y_ap, in0=on[:, 0:nb, :], in1=gain_ap, op=ALU.mult))

        def rmsnorm_tile(ph, xt, sq, hT, g_sb, rstd, ntok, psbank):
            for c in range(8):
                E = act if c % 2 == 0 else pool
                if E is act:
                    kb.op(act, [xt], [sq], lambda e: e.activation(out=sq[:, c, 0:ntok], in_=xt[:, c, 0:ntok], func=AF.Square))
                else:
                    kb.op(pool, [xt], [sq], lambda e: e.tensor_tensor(out=sq[:, c, 0:ntok], in0=xt[:, c, 0:ntok], in1=xt[:, c, 0:ntok], op=ALU.mult))
            for c in range(8):
                kb.op(pe, [sq, onesn], [psbank], lambda e: e.matmul(psbank[:, 0:ntok], lhsT=onesn[:, :], rhs=sq[:, c, 0:ntok], start=(c == 0), stop=(c == 7)))
            rsqrt(rstd, rstd[:, 0:ntok], psbank, psbank[:, 0:ntok], 1.0)
            for c in range(8):
                E = dve
                kb.op(E, [xt, rstd, g_sb], [hT], lambda e: e.scalar_tensor_tensor(out=hT[:, c, 0:ntok], in0=xt[:, c, 0:ntok], scalar=g_sb[:, c:c + 1], in1=rstd[:, 0:ntok], op0=ALU.mult, op1=ALU.mult))

        x_cur = xT_in
        for L in range(n_layers):
            with kb.phase() as lay:
                cumTM = lay.sb("cumTM", [128, 32, 6], F32)
                Rend = lay.sb("Rend", [128, 32, 6], F32)
                kcT = lay.sb("kcT", [64, 256], BF16)
                VCM = lay.sb("VCM", [128, 2, 129], BF16)
                gates_sb = lay.sb("gates_sb", [128, 32, 12], F32)
                lfT = lay.sb("lfT", [6, S], F32)
                with kb.phase() as ph:
                    winT = ph.sb("winT", [128, 8, WCOLS], BF16)
                    stage = [ph.sb("wst%d" % i, [128, WCOLS], F32) for i in range(2)]
                    load_cast(ph, winT, lambda k: winT[:, k, :], lambda k: w_in[L, k * 128:(k + 1) * 128, :], 128, WCOLS, 8, stage, [act, dve, pool, act, dve, act, dve, pool])
                    g_sb = ph.sb("g_sb", [128, 8], F32)
                    kb.dma(g_sb[:], g_attn[L], [], [g_sb])
                    gain_r = ph.sb("gain_r", [128, 384], F32)
                    kb.dma(gain_r[:], gains[L, :, 0:384], [], [gain_r])
                    nfb = ph.sb("nfb", [6, 1], F32)
                    kb.dma(nfb[:], fbias[L], [], [nfb])
                    kb.op(dve, [nfb], [nfb], lambda e: e.tensor_scalar(out=nfb[:], in0=nfb[:], scalar1=-1.0, scalar2=None, op0=ALU.mult))
                    cos = ph.sb("cos", [128, 32, 32], F32)
                    sin = ph.sb("sin", [128, 32, 32], F32)
                    dec = ph.sb("dec", [128, 6, 128], F32)
                    dqt = ph.sb("dqt", [64, 6, 128], F32)
                    dkt = ph.sb("dkt", [128, 6], F32)
                    for t, n in ((cos, "c_cos"), (sin, "c_sin"), (dec, "c_dec"), (dqt, "c_dq"), (dkt, "c_dk")):
                        kb.dma(t[:], cin[n], [], [t])
                    xts = [ph.sb("xt%d" % i, [128, 8, 512], F32) for i in range(2)]
                    sq = ph.sb("sq", [128, 8, 512], BF16)
                    hT = ph.sb("hT", [128, 8, 512], BF16)
                    rstd = ph.sb("rstd", [128, 512], F32)
                    fme = [ph.sb("fme%d" % i, [128, 512], BF16) for i in range(3)]
                    lft = ph.sb("lft", [6, 512], F32)
                    vt = [ph.sb("vt%d" % i, [128, 8, 65], BF16) for i in range(2)]
                    for v in vt:
                        kb.op(pool, [], [v], lambda e: e.memset(v[:], 1.0))
                    ra, rb, rc, rd = [ph.sb("r%s" % n, [128, 12, 32], F32) for n in "abcd"]
                    qkr = ph.sb("qkr", [128, 12, 2, 32], BF16)
                    vbf = ph.sb("vbf", [128, 384], BF16)
                    qTp = ph.sb("qTp", [64, 6, 128], BF16)
                    qTd = ph.sb("qTd", [64, 6, 128], BF16)
                    kTp = ph.sb("kTp", [64, 6, 128], BF16)
                    inn = ph.sb("inn", [128, 6, 128], BF16)
                    kd = ph.sb("kd", [128, 6, 64], BF16)
                    Sf = ph.sb("Sf", [64, 6, 64], F32)
                    Sb = ph.sb("Sb", [64, 6, 64], BF16)
                    kb.op(dve, [], [Sf], lambda e: e.memset(Sf[:], 0.0))
                    kb.op(dve, [], [Sb], lambda e: e.memset(Sb[:], 0.0))
                    of = ph.sb("of", [128, 6, 64], F32)
                    osq = ph.sb("osq", [128, 6, 64], F32)
                    sg = ph.sb("sg", [128, 384], F32)
                    st1 = ph.sb("st1", [128, 6], F32)
                    st2 = ph.sb("st2", [128, 6], F32)
                    st3 = ph.sb("st3", [128, 6], F32)
                    yr = [ph.sb("yr%d" % i, [128, 384], BF16) for i in range(2)]

                    def load_x(Tt):
                        xt = xts[Tt % 2]
                        kb.dma(xt[:], x_cur[:, :].rearrange("(c p) t -> p c t", p=128)[:, :, Tt * 512:(Tt + 1) * 512], [x_cur], [xt])

                    load_x(0)
                    for Tt in range(8):
                        xt = xts[Tt % 2]
                        if Tt + 1 < 8:
                            load_x(Tt + 1)
                        rmsnorm_tile(ph, xt, sq, hT, g_sb, rstd, 512, ps[0])
                        def fm_chunk(j, bank):
                            m = 128 if j < 10 else 6
                            for c in range(8):
                                kb.op(pe, [winT, hT], [bank], lambda e: e.matmul(bank[0:m, :], lhsT=winT[:, c, FM0 + j * 128:FM0 + j * 128 + m], rhs=hT[:, c, :], start=(c == 0), stop=(c == 7)))
                            if j < 10:
                                fe = fme[j % 3]
                                scale = 0.125 if (j < 3 or j in (6, 7)) else 1.0
                                kb.op(act, [bank], [fe], lambda e: e.activation(out=fe[:], in_=bank[:, :], func=AF.Copy, scale=scale))
                                kb.dma(FMd[j * 128:(j + 1) * 128, Tt * 512:(Tt + 1) * 512], fe[:], [fe], [FMd])
                            else:
                                kb.op(act, [bank, nfb], [lft], lambda e: e.activation(out=lft[:], in_=bank[0:6, :], func=AF.Exp, bias=nfb[:], scale=-1.0))
                                kb.op(act, [lft], [lfT], lambda e: e.activation(out=lfT[:, Tt * 512:(Tt + 1) * 512], in_=lft[:], func=AF.Ln, bias=1.0, scale=1.0))
                        fm_chunk(0, ps[1])
                        fm_chunk(1, ps[2])
                        for sub in range(4):
                            cc = Tt * 4 + sub
                            tk = slice(sub * 128, (sub + 1) * 128)
                            groups = [(ps[3], TM_RQ, 384), (ps[4], TM_RK, 384), (ps[5], TM_RV, 512), (ps[0], TM_RG, 396), (ps[1 + (sub % 2)], TM_FV, 384)]
                            for bank, c0, ncol in groups:
                                for c in range(8):
                                    kb.op(pe, [winT, hT], [bank], lambda e: e.matmul(bank[:, 0:ncol], lhsT=hT[:, c, tk], rhs=winT[:, c, c0:c0 + ncol], start=(c == 0), stop=(c == 7)))
                            for j_ in {0: (2, 3, 4), 1: (5, 6), 2: (7, 8), 3: (9, 10)}[sub]:
                                fm_chunk(j_, ps[1 + ((sub + 1) % 2)])
                            pq, pk, pv, pg, pf = ps[3], ps[4], ps[5], ps[0], ps[1 + (sub % 2)]
                            v_t = vt[cc % 2]
                            kb.op(act, [pf], [v_t], lambda e: e.activation(out=v_t[:, 0:6, 0:64], in_=pf[:, 0:384].rearrange("p (h d) -> p h d", d=64), func=AF.Copy))
                            kb.op(act, [pv], [v_t], lambda e: e.activation(out=v_t[:, 6:8, 0:64], in_=pv[:, 384:512].rearrange("p (h d) -> p h d", d=64), func=AF.Copy))
                            kb.dma(VTM[cc * 128:(cc + 1) * 128, :], v_t[:].rearrange("p h d -> p (h d)"), [v_t], [VTM])
                            kb.op(act, [pv], [vbf], lambda e: e.activation(out=vbf[:], in_=pv[:, 0:384], func=AF.Copy))
                            kb.op(act, [pg], [gates_sb], lambda e: e.activation(out=gates_sb[:, cc, :], in_=pg[:, 384:396], func=AF.Sigmoid))
                            kb.op(act, [pg], [sg], lambda e: e.activation(out=sg[:], in_=pg[:, 0:384], func=AF.Silu))
                            cosb = cos[:, cc:cc + 1, :].to_broadcast([128, 6, 32])
                            sinb = sin[:, cc:cc + 1, :].to_broadcast([128, 6, 32])
                            for qi, pb in enumerate((pq, pk)):
                                v4 = pb[:, 0:384].rearrange("p (h two f) -> p h two f", two=2, f=32)
                                hs = slice(qi * 6, qi * 6 + 6)
                                kb.op(dve, [pb, cos], [ra], lambda e: e.tensor_tensor(out=ra[:, hs, :], in0=v4[:, :, 0, :], in1=cosb, op=ALU.mult))
                                kb.op(dve, [pb, sin], [rb], lambda e: e.tensor_tensor(out=rb[:, hs, :], in0=v4[:, :, 1, :], in1=sinb, op=ALU.mult))
                                kb.op(dve, [pb, sin], [rc], lambda e: e.tensor_tensor(out=rc[:, hs, :], in0=v4[:, :, 0, :], in1=sinb, op=ALU.mult))
                                kb.op(dve, [pb, cos], [rd], lambda e: e.tensor_tensor(out=rd[:, hs, :], in0=v4[:, :, 1, :], in1=cosb, op=ALU.mult))
                            kb.op(pool, [ra, rb], [qkr], lambda e: e.tensor_tensor(out=qkr[:, :, 0, :], in0=ra[:], in1=rb[:], op=ALU.subtract))
                            kb.op(pool, [rc, rd], [qkr], lambda e: e.tensor_tensor(out=qkr[:, :, 1, :], in0=rc[:], in1=rd[:], op=ALU.add))
                            qk2 = qkr[:].rearrange("p h two f -> p (h two f)")
                            for i in range(12):
                                dstb = psb if i < 6 else psb2
                                kb.op(pe, [qkr, identb], [dstb], lambda e: e.transpose(dstb[0:64, (i % 6) * 128:(i % 6 + 1) * 128], qk2[:, i * 64:(i + 1) * 64], identb[:, :]))
                            pTq = psb[0:64, 0:768].rearrange("p (i n) -> p i n", n=128)
                            pTk = psb2[0:64, 0:768].rearrange("p (i n) -> p i n", n=128)
                            kb.op(act, [psb], [qTp], lambda e: e.activation(out=qTp[:], in_=pTq, func=AF.Copy))
                            kb.op(dve, [psb, dqt], [qTd], lambda e: e.tensor_tensor(out=qTd[:], in0=pTq, in1=dqt[:], op=ALU.mult))
                            kb.op(act, [psb2], [kTp], lambda e: e.activation(out=kTp[:], in_=pTk, func=AF.Copy))
                            for h in range(6):
                                bank = ps[3] if h < 3 else ps[4]
                                kb.op(pe, [kTp, qTp], [bank], lambda e: e.matmul(bank[:, (h % 3) * 128:(h % 3 + 1) * 128], lhsT=kTp[:, h, :], rhs=qTp[:, h, :], start=True, stop=True))
                            for half in range(2):
                                bank = ps[3 + half]
                                kb.op(dve, [bank, dec], [inn], lambda e: e.tensor_tensor(out=inn[:, half * 3:half * 3 + 3, :], in0=bank[:, 0:384].rearrange("p (h n) -> p h n", n=128), in1=dec[:, half * 3:half * 3 + 3, :], op=ALU.mult))
                            po = ps[0]
                            for h in range(6):
                                if cc > 0:
                                    kb.op(pe, [qTd, Sb], [po], lambda e: e.matmul(po[:, h * 64:(h + 1) * 64], lhsT=qTd[:, h, :], rhs=Sb[:, h, :], start=True, stop=False))
                                kb.op(pe, [inn, vbf], [po], lambda e: e.matmul(po[:, h * 64:(h + 1) * 64], lhsT=inn[:, h, :], rhs=vbf[:, h * 64:(h + 1) * 64], start=(cc == 0), stop=True))
                            if cc < 31:
                                kb.op(pool, [qkr, dkt], [kd], lambda e: e.tensor_tensor(out=kd[:], in0=qkr[:, 6:12, :, :].rearrange("p h two f -> p h (two f)"), in1=dkt[:, :].unsqueeze(2).to_broadcast([128, 6, 64]), op=ALU.mult))
                                pst = ps[5]
                                for h in range(6):
                                    kb.op(pe, [kd, vbf], [pst], lambda e: e.matmul(pst[0:64, h * 64:(h + 1) * 64], lhsT=kd[:, h, :], rhs=vbf[:, h * 64:(h + 1) * 64], start=True, stop=True))
                                for h in range(6):
                                    gC = float(np.exp(np.log(1.0 - 2.0 ** (-5.0 - h)) * 128.0))
                                    kb.op(dve, [pst, Sf], [Sf], lambda e: e.scalar_tensor_tensor(out=Sf[:, h, :], in0=Sf[:, h, :], scalar=gC, in1=pst[0:64, h * 64:(h + 1) * 64], op0=ALU.mult, op1=ALU.add))
                                kb.op(pool, [Sf], [Sb], lambda e: e.tensor_copy(out=Sb[:], in_=Sf[:]))
                            kb.op(act, [po], [of], lambda e: e.activation(out=of[:], in_=po[:, 0:384].rearrange("p (h d) -> p h d", d=64), func=AF.Copy))
                            kb.op(dve, [of], [st1], lambda e: e.tensor_reduce(out=st1[:], in_=of[:], axis=AX.X, op=ALU.add))
                            kb.op(pool, [of], [osq], lambda e: e.tensor_tensor(out=osq[:], in0=of[:], in1=of[:], op=ALU.mult))
                            kb.op(dve, [osq], [st2], lambda e: e.tensor_reduce(out=st2[:], in_=osq[:], axis=AX.X, op=ALU.add))
                            kb.op(dve, [st1], [st1], lambda e: e.tensor_scalar(out=st1[:], in0=st1[:], scalar1=1.0 / 64.0, scalar2=None, op0=ALU.mult))
                            kb.op(dve, [st1], [st3], lambda e: e.tensor_tensor(out=st3[:], in0=st1[:], in1=st1[:], op=ALU.mult))
                            kb.op(dve, [st2, st3], [st2], lambda e: e.scalar_tensor_tensor(out=st2[:], in0=st2[:], scalar=1.0 / 64.0, in1=st3[:], op0=ALU.mult, op1=ALU.subtract))
                            rsqrt(st2, st2[:], st2, st2[:], 1.0)
                            kb.op(dve, [of, st1], [of], lambda e: e.tensor_tensor(out=of[:], in0=of[:], in1=st1[:, :].unsqueeze(2).to_broadcast([128, 6, 64]), op=ALU.subtract))
                            kb.op(dve, [of, st2], [of], lambda e: e.tensor_tensor(out=of[:], in0=of[:], in1=st2[:, :].unsqueeze(2).to_broadcast([128, 6, 64]), op=ALU.mult))
                            of2 = of[:].rearrange("p h d -> p (h d)")
                            kb.op(pool, [of, gain_r], [of], lambda e: e.tensor_tensor(out=of2, in0=of2, in1=gain_r[:], op=ALU.mult))
                            y_r = yr[cc % 2]
                            kb.op(pool, [of, sg], [y_r], lambda e: e.tensor_tensor(out=y_r[:], in0=of2, in1=sg[:], op=ALU.mult))
                            kb.dma(Yd[cc * 128:(cc + 1) * 128, 0:384], y_r[:], [y_r], [Yd])
                    if debug:
                        kb.dma(dbg_lf[:, :], lfT[:], [lfT], [dbg_lf])
                        kb.dma(dbg_gates[:, :, :], gates_sb[:], [gates_sb], [dbg_gates])

                if debug == "P":
                    break

                with kb.phase() as ph:
                    cumL = ph.sb("cumL", [6, S], F32)
                    z6 = ph.sb("z6", [6, 512], F32)
                    kb.op(dve, [], [z6], lambda e: e.memset(z6[:], 0.0))
                    for j in range(8):
                        sl = slice(j * 512, (j + 1) * 512)
                        init = 0.0 if j == 0 else cumL[:, j * 512 - 1:j * 512]
                        kb.op(dve, [lfT, z6, cumL], [cumL], lambda e: e.tensor_tensor_scan(out=cumL[:, sl], data0=lfT[:, sl], data1=z6[:], initial=init, op0=ALU.add, op1=ALU.add))
                    dqf = ph.sb("dqf", [6, S], F32)
                    dqh = ph.sb("dqh", [6, S], BF16)
                    dql = ph.sb("dql", [6, S], BF16)
                    for j in range(8):
                        sl = slice(j * 512, (j + 1) * 512)
                        if j == 0:
                            kb.op(dve, [cumL], [dqf], lambda e: e.tensor_scalar(out=dqf[:, sl], in0=cumL[:, sl], scalar1=-1.0, scalar2=None, op0=ALU.mult))
                        else:
                            kb.op(dve, [cumL], [dqf], lambda e: e.tensor_scalar(out=dqf[:, sl], in0=cumL[:, sl], scalar1=-1.0, scalar2=cumL[:, j * 512 - 1:j * 512], op0=ALU.mult, op1=ALU.add))
                    kb.op(dve, [dqf], [dqh], lambda e: e.tensor_copy(out=dqh[:], in_=dqf[:]))
                    kb.op(dve, [dqf, dqh], [dql], lambda e: e.tensor_tensor(out=dql[:], in0=dqf[:], in1=dqh[:], op=ALU.subtract))
                    kb.dma(DQd[0], dqh[:], [dqh], [DQd])
                    kb.dma(DQd[1], dql[:], [dql], [DQd])
                    pc, pr_ = ps[0], ps[1]
                    for c in range(32):
                        kb.op(pe, [cumL, identf], [pc], lambda e: e.transpose(pc[:, c * 6:(c + 1) * 6], cumL[:, c * 128:(c + 1) * 128], identf[0:6, 0:6]))
                        kb.op(pe, [cumL, identf], [pr_], lambda e: e.transpose(pr_[:, c * 6:(c + 1) * 6], cumL[:, c * 128 + 127:c * 128 + 128].to_broadcast([6, 128]), identf[0:6, 0:6]))
                    kb.op(act, [pc], [cumTM], lambda e: e.activation(out=cumTM[:].rearrange("p c h -> p (c h)"), in_=pc[:, 0:192], func=AF.Copy))
                    kb.op(act, [pr_], [Rend], lambda e: e.activation(out=Rend[:].rearrange("p c h -> p (c h)"), in_=pr_[:, 0:192], func=AF.Copy))

                with kb.phase() as ph:
                    kT = [ph.sb("fkT%d" % h, [66, S], BF16) for h in range(6)]
                    for h in range(6):
                        kb.dma(kT[h][0:64, :], FMd[FM_FK + h * 64:FM_FK + (h + 1) * 64, :], [FMd], [kT[h]])
                        kb.op(pool, [], [kT[h]], lambda e: e.memset(kT[h][64:66, :], 1.0))
                    vF = ph.sb("vF", [128, 32, 390], BF16)
                    kb.dma(vF[:], VTM[:, 0:390].rearrange("(c p) f -> p c f", p=128), [VTM], [vF])
                    gain_f = ph.sb("gain_f", [128, 384], F32)
                    kb.dma(gain_f[:], gains[L, :, 384:768], [], [gain_f])
                    fmask = ph.sb("fmask", [128, 4, 512], BF16)
                    kb.dma(fmask[:], cin["c_fmask"], [], [fmask])
                    qts = [ph.sb("fq%d" % i, [66, 6, 512], BF16) for i in range(2)]
                    pts = [ph.sb("fpt%d" % i, [128, 512], BF16) for i in range(3)]
                    ots = [ph.sb("fot%d" % i, [65, 512], F32) for i in range(2)]
                    yts = [ph.sb("fy%d" % i, [128, 4, 384], BF16) for i in range(2)]
                    on = ph.sb("fon", [128, 4, 64], F32)
                    osq = ph.sb("fosq", [128, 4, 64], F32)
                    rinv = ph.sb("frinv", [128, 4], F32)
                    ss = ph.sb("fss", [128, 4], F32)

                    def load_q(j):
                        qt = qts[j % 2]
                        kb.dma(qt[0:64, :, :], FMd[0:384, j * 512:(j + 1) * 512].rearrange("(h d) t -> d h t", d=64), [FMd], [qt])
                        kb.dma(qt[64:66, :, :], DQd[:, :, j * 512:(j + 1) * 512], [DQd], [qt])

                    biases = [ph.sb("fbj%d" % j, [128, 32, 6], F32) for j in range(8)]
                    for j in range(8):
                        nch = 4 * j + 4
                        bj = biases[j]
                        if j == 0:
                            kb.op(pool, [cumTM], [bj], lambda e: e.tensor_copy(out=bj[:, 0:nch, :], in_=cumTM[:, 0:nch, :]))
                        else:
                            kb.op(dve, [cumTM, Rend], [bj], lambda e: e.tensor_tensor(out=bj[:, 0:nch, :], in0=cumTM[:, 0:nch, :], in1=Rend[:, 4 * j - 1:4 * j, :].to_broadcast([128, nch, 6]), op=ALU.subtract))
                    load_q(0)
                    items = [(j, h, c) for j in range(8) for h in range(6) for c in range(4 * j + 4)]
                    n_it = len(items)
                    deferred = []

                    def emit_qk(k):
                        j, h, c = items[k]
                        if h == 0 and c == 0 and j + 1 < 8:
                            load_q(j + 1)
                        qt = qts[j % 2]
                        i = c - 4 * j
                        c0 = 128 * i if i > 0 else 0
                        diag = i >= 0
                        sb_ = ps[1 + (k % 3)]
                        kb.op(pe, [kT[h], qt], [sb_], lambda e: e.matmul(sb_[:, c0:512], lhsT=kT[h][0:66, c * 128:(c + 1) * 128], rhs=qt[0:66, h, c0:512], start=True, stop=not diag))
                        if diag:
                            kb.op(pe, [identb, fmask], [sb_], lambda e: e.matmul(sb_[:, c0:512], lhsT=identb[:, :], rhs=fmask[:, i, c0:512], start=False, stop=True))

                    def finish_b(j, h):
                        ot = ots[h % 2]
                        yt = yts[j % 2]
                        ptr = ps[0]
                        for bq in range(4):
                            kb.op(pe, [ot, identf], [ptr], lambda e: e.transpose(ptr[:, bq * 65:(bq + 1) * 65], ot[0:65, bq * 128:(bq + 1) * 128], identf[0:65, 0:65]))
                        o3 = ptr[:, 0:260].rearrange("p (b f) -> p b f", f=65)
                        head_finish(o3, 4, on, osq, rinv, ss, gain_f[:, h * 64:(h + 1) * 64].unsqueeze(1).to_broadcast([128, 4, 64]), yt, yt[:, :, h * 64:(h + 1) * 64], ptr, gain_f)
                        if h == 5:
                            kb.dma(Yd[j * 512:(j + 1) * 512, 384:768].rearrange("(b p) f -> p b f", p=128), yt[:], [yt], [Yd])

                    def emit_exp_pv(k):
                        j, h, c = items[k]
                        nch = 4 * j + 4
                        i = c - 4 * j
                        c0 = 128 * i if i > 0 else 0
                        sb_ = ps[1 + (k % 3)]
                        pt = pts[k % 3]
                        po = ps[4 + ((j * 6 + h) % 2)]
                        bj = biases[j]
                        kb.op(act, [sb_, bj], [pt], lambda e: e.activation(out=pt[:, c0:512], in_=sb_[:, c0:512], func=AF.Exp, bias=bj[:, c, h:h + 1], scale=1.0))
                        kb.op(pe, [vF, pt], [po], lambda e: e.matmul(po[0:65, c0:512], lhsT=vF[:, c, h * 65:(h + 1) * 65], rhs=pt[:, c0:512], start=(c == 0), stop=(c == nch - 1)))
                        if c == nch - 1:
                            ot = ots[h % 2]
                            kb.op(dve, [po], [ot], lambda e: e.tensor_copy(out=ot[:], in_=po[0:65, :]))
                            deferred.append((k + 2, j, h))

                    emit_qk(0)
                    if n_it > 1:
                        emit_qk(1)
                    for k in range(n_it):
                        if k + 2 < n_it:
                            emit_qk(k + 2)
                        emit_exp_pv(k)
                        while deferred and deferred[0][0] <= k:
                            _, dj, dh = deferred.pop(0)
                            finish_b(dj, dh)
                    while deferred:
                        _, dj, dh = deferred.pop(0)
                        finish_b(dj, dh)
                if debug == "F":
                    break

                with kb.phase() as ph:
                    kb.op(pool, [], [VCM], lambda e: e.memset(VCM[:], 0.0))
                    kb.op(pool, [], [kcT], lambda e: e.memset(kcT[:], 0.0))
                    m_st = ph.sb("m_st", [128, 2, 64], BF16)
                    kb.dma(m_st[:], cin["c_cmp2sel"], [], [m_st])
                    kb.op(pool, [m_st], [VCM], lambda e: e.tensor_copy(out=VCM[:, :, 64:128], in_=m_st[:]))
                    kb.op(pool, [], [VCM], lambda e: e.memset(VCM[:, :, 128:129], 1.0))
                    hid = {}
                    w2s = {}
                    for nm, w1d, w2d, posd, row0 in (("k", w1k, w2k, posk, FM_NKC), ("v", w1v, w2v, posv, FM_NVC)):
                        w1st = ph.sb("w1st" + nm, [64, 32, 256], F32)
                        w1b = ph.sb("w1b" + nm, [64, 32, 256], BF16)
                        kb.dma(w1st[:], w1d[L].rearrange("(l d) j -> d l j", d=64), [], [w1st])
                        kb.op(pool if nm == "k" else dve, [w1st], [w1b], lambda e: e.tensor_copy(out=w1b[:], in_=w1st[:]))
                        w2st = ph.sb("w2st" + nm, [128, 2, 64], F32)
                        w2b = ph.sb("w2b" + nm, [128, 2, 64], BF16)
                        kb.dma(w2st[:], w2d[L].rearrange("(c p) e -> p c e", p=128), [], [w2st])
                        kb.op(dve, [w2st], [w2b], lambda e: e.tensor_copy(out=w2b[:], in_=w2st[:]))
                        w2s[nm] = w2b
                        pos = ph.sb("pos" + nm, [64, 32], F32)
                        kb.dma(pos[:], posd[L], [], [pos])
                        kvT = ph.sb("kvT" + nm, [64, S], BF16)
                        kb.dma(kvT[:], FMd[row0:row0 + 64, :], [FMd], [kvT])
                        blk = ph.sb("blk" + nm, [64, 32, 255], BF16)
                        v3 = kvT[:, :].rearrange("d (i r) -> d i r", r=16)
                        kb.op(dve, [kvT, pos], [blk], lambda e: e.tensor_tensor(out=blk[:, 0:16, :], in0=v3[:, 0:255, :].rearrange("d i r -> d r i"), in1=pos[:, 0:16].unsqueeze(2).to_broadcast([64, 16, 255]), op=ALU.add))
                        kb.op(pool, [kvT, pos], [blk], lambda e: e.tensor_tensor(out=blk[:, 16:32, :], in0=v3[:, 1:256, :].rearrange("d i r -> d r i"), in1=pos[:, 16:32].unsqueeze(2).to_broadcast([64, 16, 255]), op=ALU.add))
                        hb = ph.sb("hid" + nm, [128, 2, 256], BF16)
                        kb.op(pool, [], [hb], lambda e: e.memset(hb[:], 0.0))
                        xs = ph.sb("gx" + nm, [128, 255], F32)
                        x2 = ph.sb("gx2" + nm, [128, 255], F32)
                        for jc in range(2):
                            bank = ps[1 + jc]
                            for l in range(32):
                                kb.op(pe, [w1b, blk], [bank], lambda e: e.matmul(bank[:, 0:255], lhsT=w1b[:, l, jc * 128:(jc + 1) * 128], rhs=blk[:, l, :], start=(l == 0), stop=(l == 31)))
                            kb.op(act, [bank], [xs], lambda e: e.activation(out=xs[:], in_=bank[:, 0:255], func=AF.Copy))
                            kb.op(pool, [xs], [x2], lambda e: e.tensor_tensor(out=x2[:], in0=xs[:], in1=xs[:], op=ALU.mult))
                            kb.op(dve, [x2], [x2], lambda e: e.tensor_scalar(out=x2[:], in0=x2[:], scalar1=0.044715, scalar2=1.0, op0=ALU.mult, op1=ALU.add))
                            kb.op(pool, [x2, xs], [x2], lambda e: e.tensor_tensor(out=x2[:], in0=x2[:], in1=xs[:], op=ALU.mult))
                            kb.op(act, [x2], [x2], lambda e: e.activation(out=x2[:], in_=x2[:], func=AF.Sigmoid, scale=1.5957691216057308))
                            kb.op(pool, [x2, xs], [hb], lambda e: e.tensor_tensor(out=hb[:, jc, 0:255], in0=x2[:], in1=xs[:], op=ALU.mult))
                        hid[nm] = hb
                    pk_ = ps[3]
                    for jc in range(2):
                        kb.op(pe, [w2s["k"], hid["k"]], [pk_], lambda e: e.matmul(pk_[0:64, 0:255], lhsT=w2s["k"][:, jc, :], rhs=hid["k"][:, jc, 0:255], start=(jc == 0), stop=(jc == 1)))
                    kb.op(act, [pk_], [kcT], lambda e: e.activation(out=kcT[:, 0:255], in_=pk_[0:64, 0:255], func=AF.Copy))
                    pv_ = ps[4]
                    for n_ in range(2):
                        nn = 128 if n_ == 0 else 127
                        for jc in range(2):
                            kb.op(pe, [w2s["v"], hid["v"]], [pv_], lambda e: e.matmul(pv_[0:nn, n_ * 64:(n_ + 1) * 64], lhsT=hid["v"][:, jc, n_ * 128:n_ * 128 + nn], rhs=w2s["v"][:, jc, :], start=(jc == 0), stop=(jc == 1)))
                        kb.op(act, [pv_], [VCM], lambda e: e.activation(out=VCM[0:nn, n_, 0:64], in_=pv_[0:nn, n_ * 64:(n_ + 1) * 64], func=AF.Copy))

                with kb.phase() as ph:
                    ksA = ph.sb("ksA", [128, S], BF16)
                    kb.dma(ksA[0:64, :], FMd[FM_NKS:FM_NKS + 64, :], [FMd], [ksA])
                    kb.dma(ksA[64:128, :], cin["c_expand"], [], [ksA])
                    kwT = ph.sb("kwT", [64, S], BF16)
                    kb.dma(kwT[:], FMd[FM_NKW:FM_NKW + 64, :], [FMd], [kwT])
                    vN = ph.sb("vN", [128, 32, 130], BF16)
                    kb.dma(vN[:], VTM[:, 390:520].rearrange("(c p) f -> p c f", p=128), [VTM], [vN])
                    cmpmask = ph.sb("cmpmask", [128, 2, S], BF16)
                    kb.dma(cmpmask[:], cin["c_cmpmask"], [], [cmpmask])
                    addc = ph.sb("addc", [128, 32, 64], F32)
                    multm = ph.sb("multm", [128, 32, 64], F32)
                    tri = ph.sb("tri", [128, 4, 128], BF16)
                    tri2 = ph.sb("tri2", [128, 4, 128], BF16)
                    gain_n = ph.sb("gain_n", [128, 256], F32)
                    for t_, n_ in ((addc, "c_addc"), (multm, "c_mult"), (tri, "c_tri"), (tri2, "c_tri2")):
                        kb.dma(t_[:], cin[n_], [], [t_])
                    kb.dma(gain_n[:], gains[L, :, 768:1024], [], [gain_n])
                    qAs = [ph.sb("qA%d" % i, [128, 4, 128], BF16) for i in range(2)]
                    Et = ph.sb("Et", [128, 2, 512], BF16)
                    pts = [ph.sb("npt%d" % i, [128, 512], BF16) for i in range(3)]
                    rzc = ph.sb("rzc", [128, 4], F32)
                    imp = ph.sb("imp", [128, 64], F32)
                    score = ph.sb("score", [128, 64], F32)
                    sc2 = ph.sb("sc2", [128, 64], F32)
                    m8 = ph.sb("m8", [128, 8], F32)
                    m8b = ph.sb("m8b", [128, 8], F32)
                    negm = ph.sb("negm", [128, 64], BF16)
                    zall = ph.sb("zall", [128, 4, 3], F32)
                    coef = ph.sb("coef", [128, 4, 3], F32)
                    on = ph.sb("non", [128, 4, 64], F32)
                    osq = ph.sb("nosq", [128, 4, 64], F32)
                    ss = ph.sb("nss", [128, 4], F32)
                    yns = [ph.sb("yn%d" % i, [128, 4, 64], BF16) for i in range(2)]
                    tri_2d = tri[:].rearrange("p h t -> p (h t)")
                    tri2_2d = tri2[:].rearrange("p h t -> p (h t)")

                    def load_qa(b):
                        qa = qAs[b % 2]
                        kb.dma(qa[0:64, :, :], FMd[FM_NQ:FM_NQ + 256, b * 128:(b + 1) * 128].rearrange("(h d) t -> d h t", d=64), [FMd], [qa])

                    on_cs = [ph.sb("onc%d" % i, [128, 4, 64], F32) for i in range(2)]
                    gco = ph.sb("gco", [128, 4], F32)
                    ots_s = ph.sb("ots_s", [65, 512], F32)
                    ots_w = ph.sb("ots_w", [65, 512], F32)
                    zc4 = ph.sb("zc4", [128, 4], F32)
                    pos_ = ps[5]
                    pow_ = ps[0]
                    cnt_box = [0]

                    def oc_views():
                        return [ps[3 + k][:, 0:258].rearrange("p (h f) -> p h f", f=129) for k in range(2)]

                    def cmp_pe1(b):
                        qa = qAs[b % 2]
                        q2 = qa[:].rearrange("p h t -> p (h t)")
                        ncn = 2 if b >= 16 else 1
                        for n_ in range(ncn):
                            pse = ps[1 + n_]
                            kb.op(pe, [kcT, qa], [pse], lambda e: e.matmul(pse[:, :], lhsT=kcT[0:64, n_ * 128:(n_ + 1) * 128], rhs=q2[0:64, :], start=True, stop=True))
                            kb.op(act, [pse], [Et], lambda e: e.activation(out=Et[:, n_, :], in_=pse[:, :], func=AF.Exp))
                            kb.op(pool, [Et, cmpmask], [Et], lambda e: e.tensor_tensor(out=Et[:, n_, :].rearrange("p (h t) -> p h t", t=128), in0=Et[:, n_, :].rearrange("p (h t) -> p h t", t=128), in1=cmpmask[:, n_, b * 128:(b + 1) * 128].unsqueeze(1).to_broadcast([128, 4, 128]), op=ALU.mult))

                    def cmp_pe2(b):
                        ncn = 2 if b >= 16 else 1
                        for h in range(4):
                            bank = ps[3 + h // 2]
                            off = (h % 2) * 129
                            for n_ in range(ncn):
                                kb.op(pe, [Et, VCM], [bank], lambda e: e.matmul(bank[:, off:off + 129], lhsT=Et[:, n_, h * 128:(h + 1) * 128], rhs=VCM[:, n_, :], start=(n_ == 0), stop=(n_ == ncn - 1)))

                    def chain(b):
                        oc = oc_views()
                        for k in range(2):
                            kb.op(dve, [ps[3 + k]], [zc4], lambda e: e.tensor_scalar(out=zc4[:, 2 * k:2 * k + 2], in0=oc[k][:, :, 128], scalar1=1e-30, scalar2=None, op0=ALU.add))
                        kb.op(dve, [zc4], [rzc], lambda e: e.reciprocal(out=rzc[:], in_=zc4[:]))
                        for h in range(4):
                            src = oc[h // 2][:, h % 2, 64:128]
                            if h == 0:
                                kb.op(dve, [ps[3], rzc], [imp], lambda e: e.tensor_scalar(out=imp[:], in0=src, scalar1=rzc[:, 0:1], scalar2=None, op0=ALU.mult))
                            else:
                                kb.op(dve, [ps[3 + h // 2], rzc, imp], [imp], lambda e: e.scalar_tensor_tensor(out=imp[:], in0=src, scalar=rzc[:, h:h + 1], in1=imp[:], op0=ALU.mult, op1=ALU.add))
                        kb.op(dve, [imp, multm], [score], lambda e: e.tensor_tensor(out=score[:], in0=imp[:], in1=multm[:, b, :], op=ALU.mult))
                        kb.op(dve, [score, addc], [score], lambda e: e.tensor_tensor(out=score[:], in0=score[:], in1=addc[:, b, :], op=ALU.add))
                        kb.op(dve, [score], [m8], lambda e: e.max(out=m8[:], in_=score[:]))
                        kb.op(dve, [score, m8], [sc2], lambda e: e.match_replace(out=sc2[:], in_to_replace=m8[:], in_values=score[:], imm_value=-1e9))
                        kb.op(dve, [sc2], [m8b], lambda e: e.max(out=m8b[:], in_=sc2[:]))
                        kb.op(dve, [score, m8b], [negm], lambda e: e.tensor_scalar(out=negm[:], in0=score[:], scalar1=m8b[:, 7:8], scalar2=NEG, op0=ALU.is_lt, op1=ALU.mult))
                        kb.op(dve, [rzc, gates_sb], [gco], lambda e: e.tensor_tensor(out=gco[:], in0=rzc[:], in1=gates_sb[:, b, :].rearrange("p (h g) -> p h g", g=3)[:, :, 0], op=ALU.mult))
                        onc = on_cs[b % 2]
                        for k in range(2):
                            kb.op(dve, [ps[3 + k], gco], [onc], lambda e: e.tensor_tensor(out=onc[:, 2 * k:2 * k + 2, :], in0=oc[k][:, :, 0:64], in1=gco[:, 2 * k:2 * k + 2].unsqueeze(2).to_broadcast([128, 2, 64]), op=ALU.mult))

                    def tr_copy(b):
                        qa = qAs[b % 2]
                        kb.op(pe, [negm, identb], [psb], lambda e: e.transpose(psb[0:64, 0:128], negm[:, :], identb[:, :]))
                        kb.op(act, [psb], [qa], lambda e: e.activation(out=qa[64:128, :, :], in_=psb[0:64, 0:128].unsqueeze(1).to_broadcast([64, 4, 128]), func=AF.Copy))

                    def qk_item(b, it):
                        qa = qAs[b % 2]
                        q2 = qa[:].rearrange("p h t -> p (h t)")
                        kind, c = it["kind"], it["c"]
                        sb_ = (ps[1], ps[2], ps6)[it["slot"] % 3]
                        if kind == "w":
                            edge = (c == b) or (c == b - 4)
                            kb.op(pe, [kwT, qa], [sb_], lambda e: e.matmul(sb_[:, :], lhsT=kwT[0:64, c * 128:(c + 1) * 128], rhs=q2[0:64, :], start=True, stop=not edge))
                            if edge:
                                mt, m2d = (tri, tri_2d) if c == b else (tri2, tri2_2d)
                                kb.op(pe, [identb, mt], [sb_], lambda e: e.matmul(sb_[:, :], lhsT=identb[:, :], rhs=m2d, start=False, stop=True))
                        else:
                            kb.op(pe, [ksA, qa], [sb_], lambda e: e.matmul(sb_[:, :], lhsT=ksA[:, c * 128:(c + 1) * 128], rhs=q2[:, :], start=True, stop=(c != b)))
                            if c == b:
                                kb.op(pe, [identb, tri], [sb_], lambda e: e.matmul(sb_[:, :], lhsT=identb[:, :], rhs=tri_2d, start=False, stop=True))

                    def ep_item(b, it):
                        kind, c = it["kind"], it["c"]
                        c_lo = max(0, b - 4)
                        sb_ = (ps[1], ps[2], ps6)[it["slot"] % 3]
                        pt = pts[it["slot"] % 3]
                        kb.op(act, [sb_], [pt], lambda e: e.activation(out=pt[:], in_=sb_[:, :], func=AF.Exp))
                        if kind == "w":
                            kb.op(pe, [pt, vN], [pow_], lambda e: e.matmul(pow_[0:65, :], lhsT=vN[:, c, 65:130], rhs=pt[:, :], start=(c == c_lo), stop=(c == b)))
                        else:
                            kb.op(pe, [pt, vN], [pos_], lambda e: e.matmul(pos_[0:65, :], lhsT=vN[:, c, 0:65], rhs=pt[:, :], start=(c == 0), stop=(c == b)))

                    def run_items(b, lst):
                        for it in lst:
                            it["slot"] = cnt_box[0]
                            cnt_box[0] += 1
                        for k in range(min(2, len(lst))):
                            qk_item(b, lst[k])
                        for k in range(len(lst)):
                            if k + 2 < len(lst):
                                qk_item(b, lst[k + 2])
                            ep_item(b, lst[k])

                    def combine(b):
                        kb.op(act, [pos_], [ots_s], lambda e: e.activation(out=ots_s[:], in_=pos_[0:65, :], func=AF.Copy))
                        kb.op(dve, [pow_], [ots_w], lambda e: e.tensor_copy(out=ots_w[:], in_=pow_[0:65, :]))
                        for h in range(4):
                            kb.op(pe, [ots_s, identf], [pos_], lambda e: e.transpose(pos_[:, h * 65:(h + 1) * 65], ots_s[0:65, h * 128:(h + 1) * 128], identf[0:65, 0:65]))
                        for h in range(4):
                            kb.op(pe, [ots_w, identf], [pow_], lambda e: e.transpose(pow_[:, h * 65:(h + 1) * 65], ots_w[0:65, h * 128:(h + 1) * 128], identf[0:65, 0:65]))
                        os3 = pos_[:, 0:260].rearrange("p (h f) -> p h f", f=65)
                        ow3 = pow_[:, 0:260].rearrange("p (h f) -> p h f", f=65)
                        g3 = gates_sb[:, b, :].rearrange("p (h g) -> p h g", g=3)
                        kb.op(dve, [pos_], [zall], lambda e: e.tensor_copy(out=zall[:, :, 1], in_=os3[:, :, 64]))
                        kb.op(dve, [pow_], [zall], lambda e: e.tensor_copy(out=zall[:, :, 2], in_=ow3[:, :, 64]))
                        kb.op(dve, [zall], [coef], lambda e: e.reciprocal(out=coef[:, :, 1:3], in_=zall[:, :, 1:3]))
                        kb.op(dve, [coef, gates_sb], [coef], lambda e: e.tensor_tensor(out=coef[:, :, 1:3], in0=coef[:, :, 1:3], in1=g3[:, :, 1:3], op=ALU.mult))
                        kb.op(dve, [pos_, coef], [on], lambda e: e.tensor_tensor(out=on[:], in0=os3[:, :, 0:64], in1=coef[:, :, 1:2].to_broadcast([128, 4, 64]), op=ALU.mult))
                        kb.op(dve, [pow_, coef], [osq], lambda e: e.tensor_tensor(out=osq[:], in0=ow3[:, :, 0:64], in1=coef[:, :, 2:3].to_broadcast([128, 4, 64]), op=ALU.mult))
                        kb.op(pool, [on, osq], [on], lambda e: e.tensor_tensor(out=on[:], in0=on[:], in1=osq[:], op=ALU.add))
                        onc = on_cs[b % 2]
                        kb.op(pool, [on, onc], [on], lambda e: e.tensor_tensor(out=on[:], in0=on[:], in1=onc[:], op=ALU.add))
                        yn = yns[b % 2]
                        norm_gain(4, on, osq, ss, gain_n[:].rearrange("p (h d) -> p h d", d=64), yn, yn[:], gain_n)
                        kb.dma(Yd[b * 128:(b + 1) * 128, 768:1024], yn[:].rearrange("p h d -> p (h d)"), [yn], [Yd])

                    load_qa(0)
                    cmp_pe1(0)
                    cmp_pe2(0)
                    chain(0)
                    for b in range(32):
                        if b + 1 < 32:
                            load_qa(b + 1)
                        run_items(b, [{"kind": "w", "c": c} for c in range(max(0, b - 4), b + 1)])
                        tr_copy(b)
                        sel = [{"kind": "s", "c": c} for c in range(b + 1)]
                        half = len(sel) // 2
                        run_items(b, sel[:half])
                        if b + 1 < 32:
                            cmp_pe1(b + 1)
                        run_items(b, sel[half:])
                        if b + 1 < 32:
                            cmp_pe2(b + 1)
                            chain(b + 1)
                        combine(b)
                if debug == "N":
                    break

            with kb.phase() as ph:
                woT = ph.sb("woT", [128, 8, 1024], BF16)
                stg = [ph.sb("wos%d" % i, [128, 1024], F32) for i in range(2)]
                load_cast(ph, woT, lambda k: woT[:, k, :], lambda k: w_out[L, k * 128:(k + 1) * 128, :], 128, 1024, 8, stg, [act, dve])
                oxts = [ph.sb("oxt%d" % i, [128, 8, 512], F32) for i in range(2)]
                oYs = [ph.sb("oY%d" % i, [128, 4, 1024], BF16) for i in range(2)]
                yT = ph.sb("oyT", [128, 8, 512], BF16)

                def load_o(Tt):
                    kb.dma(oxts[Tt % 2][:], x_cur[:, :].rearrange("(c p) t -> p c t", p=128)[:, :, Tt * 512:(Tt + 1) * 512], [x_cur], [oxts[Tt % 2]])
                    kb.dma(oYs[Tt % 2][:], Yd[Tt * 512:(Tt + 1) * 512, :].rearrange("(s p) f -> p s f", p=128), [Yd], [oYs[Tt % 2]])

                load_o(0)
                for Tt in range(8):
                    xt, Yt = oxts[Tt % 2], oYs[Tt % 2]
                    if Tt + 1 < 8:
                        load_o(Tt + 1)
                    for sub in range(4):
                        pb = psb if sub % 2 == 0 else psb2
                        for kc in range(8):
                            kb.op(pe, [Yt, identb], [pb], lambda e: e.transpose(pb[:, kc * 128:(kc + 1) * 128], Yt[:, sub, kc * 128:(kc + 1) * 128], identb[:, :]))
                        src = pb[:, 0:1024].rearrange("p (k t) -> p k t", t=128)
                        if sub % 2 == 0:
                            kb.op(act, [pb], [yT], lambda e: e.activation(out=yT[:, :, sub * 128:(sub + 1) * 128], in_=src, func=AF.Copy))
                        else:
                            kb.op(dve, [pb], [yT], lambda e: e.tensor_copy(out=yT[:, :, sub * 128:(sub + 1) * 128], in_=src))
                    for oc in range(8):
                        bank = ps[oc % 4]
                        for kc in range(8):
                            kb.op(pe, [woT, yT], [bank], lambda e: e.matmul(bank[:, :], lhsT=woT[:, kc, oc * 128:(oc + 1) * 128], rhs=yT[:, kc, :], start=(kc == 0), stop=(kc == 7)))
                        kb.op(dve, [bank, xt], [xt], lambda e: e.tensor_tensor(out=xt[:, oc, :], in0=bank[:, :], in1=xt[:, oc, :], op=ALU.add))
                    kb.dma(x1[:, :].rearrange("(c p) t -> p c t", p=128)[:, :, Tt * 512:(Tt + 1) * 512], xt[:], [xt], [x1])
            if debug == "O":
                break

            last = (L == n_layers - 1)
            with kb.phase() as ph:
                w1T = ph.sb("w1T", [128, 8, 4096], BF16)
                w2T = ph.sb("w2T", [128, 32, 1024], BF16)
                stg = [ph.sb("ms%d" % i, [128, 1024], F32) for i in range(4)]
                load_cast(ph, w1T, lambda k: w1T[:, k // 4, (k % 4) * 1024:(k % 4 + 1) * 1024], lambda k: w_m1[L, (k // 4) * 128:(k // 4 + 1) * 128, (k % 4) * 1024:(k % 4 + 1) * 1024], 128, 1024, 32, stg, [act, dve, act, dve, pool, act, dve])
                load_cast(ph, w2T, lambda k: w2T[:, k, :], lambda k: w_m2[L, k * 128:(k + 1) * 128, :], 128, 1024, 32, stg, [act, dve, act, dve, pool, act, dve])
                g2 = ph.sb("g2", [128, 8], F32)
                kb.dma(g2[:], g_mlp[L], [], [g2])
                gf = ph.sb("gf", [128, 8], F32)
                kb.dma(gf[:], g_fin, [], [gf])
                mxts = [ph.sb("mxt%d" % i, [128, 8, 256], F32) for i in range(2)]
                msq = ph.sb("msq", [128, 8, 256], BF16)
                mhT = ph.sb("mhT", [128, 8, 256], BF16)
                mrstd = ph.sb("mrstd", [128, 256], F32)
                hidT = ph.sb("hidT", [128, 32, 256], BF16)
                rts = [ph.sb("mrt%d" % i, [128, 256], F32) for i in range(3)]
                ofin = ph.sb("ofin", [128, 8, 256], F32) if (last and final_norm) else None

                def load_m(Tt):
                    kb.dma(mxts[Tt % 2][:], x1[:, :].rearrange("(c p) t -> p c t", p=128)[:, :, Tt * 256:(Tt + 1) * 256], [x1], [mxts[Tt % 2]])

                load_m(0)
                for Tt in range(16):
                    xt = mxts[Tt % 2]
                    if Tt + 1 < 16:
                        load_m(Tt + 1)
                    rmsnorm_tile(ph, xt, msq, mhT, g2, mrstd, 256, ps[0])
                    for f in range(32):
                        bank = ps[1 + f % 3]
                        for kc in range(8):
                            kb.op(pe, [w1T, mhT], [bank], lambda e: e.matmul(bank[:, 0:256], lhsT=w1T[:, kc, f * 128:(f + 1) * 128], rhs=mhT[:, kc, :], start=(kc == 0), stop=(kc == 7)))
                        rt = rts[f % 3]
                        kb.op(act, [bank], [rt], lambda e: e.activation(out=rt[:], in_=bank[:, 0:256], func=AF.Relu))
                        kb.op(dve if f % 2 == 0 else pool, [rt], [hidT], lambda e: e.tensor_tensor(out=hidT[:, f, :], in0=rt[:], in1=rt[:], op=ALU.mult))
                    for oc in range(8):
                        bank = ps[4 + oc % 2]
                        for f in range(32):
                            kb.op(pe, [w2T, hidT], [bank], lambda e: e.matmul(bank[:, 0:256], lhsT=w2T[:, f, oc * 128:(oc + 1) * 128], rhs=hidT[:, f, :], start=(f == 0), stop=(f == 31)))
                        kb.op(dve, [bank, xt], [xt], lambda e: e.tensor_tensor(out=xt[:, oc, :], in0=bank[:, 0:256], in1=xt[:, oc, :], op=ALU.add))
                    if last and final_norm:
                        rmsnorm_tile(ph, xt, msq, ofin, gf, mrstd, 256, ps[0])
                        kb.dma(out_T[:, :].rearrange("(c p) t -> p c t", p=128)[:, :, Tt * 256:(Tt + 1) * 256], ofin[:], [ofin], [out_T])
                    else:
                        dstx = out_T if last else xA
                        kb.dma(dstx[:, :].rearrange("(c p) t -> p c t", p=128)[:, :, Tt * 256:(Tt + 1) * 256], xt[:], [xt], [dstx])
            x_cur = xA
    return nc


def _prep_inputs(inputs):
    x = np.asarray(inputs["x"], np.float32)
    w_in = np.asarray(inputs["w_in"], np.float32)
    sizes = (384, 384, 384, 384, 384, 384, 384, 6, 256, 64, 64, 64, 64, 64, 64, 12)
    offs = np.concatenate([[0], np.cumsum(sizes)])
    names = ["rq", "rk", "rv", "rg", "fq", "fk", "fv", "ff", "nq", "nkc", "nvc", "nks", "nvs", "nkw", "nvw", "gate"]
    sl = {n: np.arange(offs[i], offs[i + 1]) for i, n in enumerate(names)}
    order = ["rq", "rk", "rv", "nvs", "nvw", "rg", "gate", "fv", "fq", "fk", "nq", "nkc", "nvc", "nks", "nkw", "ff"]
    perm = np.concatenate([sl[n] for n in order])
    common = {
        "w_in": np.ascontiguousarray(w_in[:, :, perm]),
        "w_out": np.asarray(inputs["w_out"], np.float32),
        "w_m1": np.asarray(inputs["w_mlp_in"], np.float32),
        "w_m2": np.asarray(inputs["w_mlp_out"], np.float32),
        "w1k": np.asarray(inputs["nsa_cmp_w1_k"], np.float32),
        "w1v": np.asarray(inputs["nsa_cmp_w1_v"], np.float32),
        "w2k": np.asarray(inputs["nsa_cmp_w2_k"], np.float32),
        "w2v": np.asarray(inputs["nsa_cmp_w2_v"], np.float32),
        "g_attn": np.ascontiguousarray(np.asarray(inputs["norm_attn"], np.float32).reshape(NL, 8, 128).transpose(0, 2, 1)),
        "g_mlp": np.ascontiguousarray(np.asarray(inputs["norm_mlp"], np.float32).reshape(NL, 8, 128).transpose(0, 2, 1)),
        "g_fin": np.ascontiguousarray(np.asarray(inputs["norm_final"], np.float32).reshape(8, 128).T),
        "gains": np.ascontiguousarray(np.broadcast_to(np.concatenate([np.asarray(inputs["ret_norm_gain"], np.float32), np.asarray(inputs["fox_norm_gain"], np.float32), np.asarray(inputs["nsa_norm_gain"], np.float32)], axis=1)[:, None, :], (NL, 128, 1024))),
        "fbias": np.asarray(inputs["fox_forget_bias"], np.float32).reshape(NL, 6, 1),
        "posk": np.ascontiguousarray(np.asarray(inputs["nsa_cmp_pos_k"], np.float32).transpose(0, 2, 1)),
        "posv": np.ascontiguousarray(np.asarray(inputs["nsa_cmp_pos_v"], np.float32).transpose(0, 2, 1)),
    }
    return x, common


def kernel(**inputs):
    x, common = _prep_inputs(inputs)
    nc = build()
    common.update(CONST_SPECS)
    in_maps = []
    for b in range(8):
        m = dict(common)
        m["xT"] = np.ascontiguousarray(x[b].T)
        in_maps.append(m)
    res = run_bass_kernel_spmd(nc, in_maps, core_ids=list(range(8)))
    out = np.stack([np.ascontiguousarray(r["outT"].T) for r in res.results], axis=0)
    return out.astype(np.float32)
```
